# Optimizing a Trainium2 kernel written in Bass

```python
import jax, jax.numpy as jnp
from jax import lax
import numpy as np

D_MODEL = 1024
BATCH = 8
SEQ = 4096
DEPTH = 2

N_MIXERS = 2
CONV_WIDTH = 3
N_HEADS = 16
N_KV_HEADS = 4
HEAD_DIM = D_MODEL // N_HEADS
GROUP = N_HEADS // N_KV_HEADS
AXIS_DIM = HEAD_DIM // 2
ROPE_THETA = 10000.0
Q_BLOCK = 128
GRID_W = 64
N_EXPERTS = 16
CAPACITY_FACTOR = 2
D_FF_EXPERT = 2 * D_MODEL
EPS = 1e-6

N_CONV_LAYERS = (DEPTH + 1) // 2
N_ATTN_LAYERS = DEPTH // 2

kernel_name = "hybrid_conv_attn_ec_moe_encoder"


def _rms32(x, g):
    x32 = x.astype(jnp.float32)
    return x32 * lax.rsqrt(jnp.mean(x32 * x32, axis=-1, keepdims=True) + EPS) * g.astype(jnp.float32)


def rmsnorm(x, g):
    return _rms32(x, g).astype(x.dtype)


def short_gated_conv(xn, w_in, w_conv, w_out):
    bch = xn @ w_in
    b, c, h = jnp.split(bch, 3, axis=-1)
    u = c * h
    up = jnp.pad(u, ((0, 0), (1, 1), (0, 0)))
    y = w_conv[0] * up[:, :-2] + w_conv[1] * up[:, 1:-1] + w_conv[2] * up[:, 2:]
    return (b * y) @ w_out


def axial_rope_tables(seq_len):
    rows = seq_len // GRID_W
    row = jnp.repeat(jnp.arange(rows, dtype=jnp.float32), GRID_W)
    col = jnp.tile(jnp.arange(GRID_W, dtype=jnp.float32), rows)
    inv_freq = ROPE_THETA ** (-jnp.arange(0, AXIS_DIM, 2, dtype=jnp.float32) / AXIS_DIM)
    ang_r = row[:, None] * inv_freq[None, :]
    ang_c = col[:, None] * inv_freq[None, :]
    return jnp.cos(ang_r), jnp.sin(ang_r), jnp.cos(ang_c), jnp.sin(ang_c)


def _rotate(x, cos, sin):
    x1, x2 = jnp.split(x, 2, axis=-1)
    cos = cos[None, :, None, :]
    sin = sin[None, :, None, :]
    return jnp.concatenate([x1 * cos - x2 * sin, x2 * cos + x1 * sin], axis=-1)


def apply_axial_rope(x, tables):
    cr, sr, cc, sc = tables
    return jnp.concatenate([_rotate(x[..., :AXIS_DIM], cr, sr),
                            _rotate(x[..., AXIS_DIM:], cc, sc)], axis=-1)


def gqa_axial_attention(xn, w_qkv, g_q, g_k, w_o):
    bsz, seq, _ = xn.shape
    dt = xn.dtype
    qkv = xn @ w_qkv
    nq, nk = N_HEADS * HEAD_DIM, N_KV_HEADS * HEAD_DIM
    q = qkv[..., :nq].reshape(bsz, seq, N_HEADS, HEAD_DIM)
    k = qkv[..., nq:nq + nk].reshape(bsz, seq, N_KV_HEADS, HEAD_DIM)
    v = qkv[..., nq + nk:].reshape(bsz, seq, N_KV_HEADS, HEAD_DIM)
    tables = axial_rope_tables(seq)
    q = (apply_axial_rope(_rms32(q, g_q), tables) * (HEAD_DIM ** -0.5)).astype(dt)
    k = apply_axial_rope(_rms32(k, g_k), tables).astype(dt)
    n_blocks = seq // Q_BLOCK
    qb = q.reshape(bsz, n_blocks, Q_BLOCK, N_KV_HEADS, GROUP, HEAD_DIM).transpose(1, 0, 2, 3, 4, 5)

    def block(q_blk):
        s = jnp.einsum('bqkgd,bskd->bkgqs', q_blk, k).astype(jnp.float32)
        p = jax.nn.softmax(s, axis=-1).astype(v.dtype)
        return jnp.einsum('bkgqs,bskd->bqkgd', p, v)

    o = lax.map(block, qb)
    o = o.transpose(1, 0, 2, 3, 4, 5).reshape(bsz, seq, N_HEADS * HEAD_DIM)
    return o @ w_o


def expert_choice_moe(xn, w_router, w_gate, w_up, w_down):
    bsz, seq, d = xn.shape
    cap = CAPACITY_FACTOR * seq // N_EXPERTS
    logits = jnp.einsum('bsd,de->bse', xn, w_router).astype(jnp.float32)
    aff = jax.nn.softmax(logits, axis=-1)
    gate, idx = lax.top_k(aff.transpose(0, 2, 1), cap)
    xg = jax.vmap(lambda xb, ib: xb[ib])(xn, idx)
    h = jax.nn.silu(jnp.einsum('becd,edf->becf', xg, w_gate)) * jnp.einsum('becd,edf->becf', xg, w_up)
    y = jnp.einsum('becf,efd->becd', h, w_down) * gate[..., None].astype(xn.dtype)
    return jax.vmap(lambda yb, ib: jnp.zeros((seq, d), xn.dtype).at[ib.reshape(-1)].add(yb.reshape(-1, d)))(y, idx)


def setup_inputs(seed: int = 0) -> dict:
    key = jax.random.key(seed)
    ks = jax.random.split(key, 16)
    D, E, F = D_MODEL, N_EXPERTS, D_FF_EXPERT
    nqkv = (N_HEADS + 2 * N_KV_HEADS) * HEAD_DIM
    nrm = lambda k, shape, s: jax.random.normal(k, shape, jnp.float32) * s
    return {
        "x": nrm(ks[0], (BATCH, SEQ, D), 1.0),
        "norm_mix": 1.0 + nrm(ks[1], (DEPTH, D), 0.02),
        "norm_ffn": 1.0 + nrm(ks[2], (DEPTH, D), 0.02),
        "conv_in": nrm(ks[3], (N_CONV_LAYERS, D, 3 * D), D ** -0.5),
        "conv_w": nrm(ks[4], (N_CONV_LAYERS, CONV_WIDTH, D), CONV_WIDTH ** -0.5),
        "conv_out": nrm(ks[5], (N_CONV_LAYERS, D, D), D ** -0.5),
        "attn_qkv": nrm(ks[6], (N_ATTN_LAYERS, D, nqkv), D ** -0.5),
        "attn_q_norm": 1.0 + nrm(ks[7], (N_ATTN_LAYERS, HEAD_DIM), 0.02),
        "attn_k_norm": 1.0 + nrm(ks[8], (N_ATTN_LAYERS, HEAD_DIM), 0.02),
        "attn_out": nrm(ks[9], (N_ATTN_LAYERS, N_HEADS * HEAD_DIM, D), D ** -0.5),
        "router": nrm(ks[10], (DEPTH, D, E), D ** -0.5),
        "w_gate": nrm(ks[11], (DEPTH, E, D, F), D ** -0.5),
        "w_up": nrm(ks[12], (DEPTH, E, D, F), D ** -0.5),
        "w_down": nrm(ks[13], (DEPTH, E, F, D), F ** -0.5),
        "final_norm": 1.0 + nrm(ks[14], (D,), 0.02),
    }


def reference(x, norm_mix, norm_ffn, conv_in, conv_w, conv_out, attn_qkv, attn_q_norm,
              attn_k_norm, attn_out, router, w_gate, w_up, w_down, final_norm):
    h = x
    for i in range(DEPTH):
        xn = rmsnorm(h, norm_mix[i])
        j = i // N_MIXERS
        if i % N_MIXERS == 0:
            h = h + short_gated_conv(xn, conv_in[j], conv_w[j], conv_out[j])
        else:
            h = h + gqa_axial_attention(xn, attn_qkv[j], attn_q_norm[j], attn_k_norm[j], attn_out[j])
        xn = rmsnorm(h, norm_ffn[i])
        h = h + expert_choice_moe(xn, router[i], w_gate[i], w_up[i], w_down[i])
    return rmsnorm(h, final_norm)
```

```python
import os
import numpy as np
import ml_dtypes
from contextlib import ExitStack
import concourse.bass as bass
import concourse.mybir as mybir
from concourse.bass_utils import run_bass_kernel_spmd

F32 = mybir.dt.float32
BF16 = mybir.dt.bfloat16
I32 = mybir.dt.int32
AF = mybir.ActivationFunctionType
ALU = mybir.AluOpType
AX = mybir.AxisListType

S = 4096
D = 1024
NT = 32
NE = 16
CAP = 512
FF = 2048
EPS = 1e-6
ENGS = ("pe", "act", "dve", "pool", "sp")
STAGE = 99
DEBUG = False
CUT = int(os.environ.get('CUT', '99'))


class Buf:
    __slots__ = ("name", "w", "r", "sem", "dma_total")

    def __init__(self, name):
        self.name = name
        self.w = {}
        self.r = {}
        self.sem = None
        self.dma_total = 0


class Tile:
    def __init__(self, t, name):
        self.t = t
        self.b = Buf(name)

    def __getitem__(self, idx):
        return self.t[idx]


def _b(x):
    return x.b if isinstance(x, Tile) else x


class Prog:
    def __init__(self, nc, stack):
        self.nc = nc
        self.stack = stack
        self.q = {e: [] for e in ENGS}
        self.cnt = {e: 0 for e in ENGS}
        self.esem = {e: stack.enter_context(nc.semaphore("es_" + e)) for e in ENGS}
        self.waited = {e: {} for e in ENGS}
        self.nsem = len(ENGS)
        self.nops = 0
        self.uid = 0
        self.owners = {}

    def name(self, base):
        self.uid += 1
        return "%s_%d" % (base, self.uid)

    def sb(self, st, name, shape, dt):
        return Tile(st.enter_context(self.nc.sbuf_tensor(self.name(name), list(shape), dt)), name)

    def _bufsem(self, b):
        if b.sem is None:
            b.sem = self.stack.enter_context(self.nc.semaphore(self.name("ds_" + b.name)))
            self.nsem += 1
        return b.sem

    def _collect(self, eng, reads, writes, is_dma):
        deps = {}
        own = id(self.esem[eng])

        def add(d, skip_own):
            for k, (s, v) in d.items():
                if skip_own and k == own:
                    continue
                if k not in deps or deps[k][1] < v:
                    deps[k] = (s, v)
        for b in reads:
            add(_b(b).w, skip_own=(eng == "pe") and not is_dma)
        for b in writes:
            add(_b(b).w, skip_own=not is_dma)
            add(_b(b).r, skip_own=not is_dma)
        out = []
        wd = self.waited[eng]
        for k, (s, v) in deps.items():
            if wd.get(k, 0) < v:
                wd[k] = v
                out.append((s, v))
        return out

    def op(self, eng, fn, reads=(), writes=()):
        waits = self._collect(eng, reads, writes, False)
        self.cnt[eng] += 1
        sem = self.esem[eng]
        self.q[eng].append((waits, fn, sem, 1))
        ev = (sem, self.cnt[eng])
        k = id(sem)
        for b in reads:
            _b(b).r[k] = ev
        for b in writes:
            _b(b).w[k] = ev
        self.nops += 1

    def dma(self, eng, fn, reads=(), writes=(), nodep_writes=(), owner=None):
        waits = self._collect(eng, reads, writes, True)
        tgt = _b(owner if owner is not None else (list(writes) + list(nodep_writes))[0])
        sem = self._bufsem(tgt)
        tgt.dma_total += 16
        self.owners[id(tgt)] = tgt
        self.q[eng].append((waits, fn, sem, 16))
        ev = (sem, tgt.dma_total)
        k = id(sem)
        for b in reads:
            _b(b).r[k] = ev
        for b in list(writes) + list(nodep_writes):
            _b(b).w[k] = ev
        self.nops += 1

    def wait_all(self, eng, bufs):
        waits = self._collect(eng, (), bufs, True)
        self.q[eng].append((waits, None, None, 0))

    def emit(self):
        nc = self.nc
        names = {"pe": "tensor", "act": "scalar", "dve": "vector", "pool": "gpsimd", "sp": "sync"}
        if not any(self.q[e] for e in ENGS):
            return
        wd = self.waited["sp"]
        drain = []
        for tgt in self.owners.values():
            k = id(tgt.sem)
            if wd.get(k, 0) < tgt.dma_total:
                wd[k] = tgt.dma_total
                drain.append((tgt.sem, tgt.dma_total))
        self.q["sp"].append((drain, None, None, 0))
        with nc.Block() as block:
            for e in ENGS:
                lst = self.q[e]

                def body(E, lst=lst):
                    for waits, fn, sem, inc in lst:
                        for (s, v) in waits:
                            E.wait_ge(s, v)
                        if fn is not None:
                            fn(E).then_inc(sem, inc)
                getattr(block, names[e])(body)
        self.q = {e: [] for e in ENGS}


class Ring:
    def __init__(self, items):
        self.items = items
        self.i = 0

    def next(self):
        x = self.items[self.i % len(self.items)]
        self.i += 1
        return x


def build_program(stage=STAGE, debug=DEBUG, only=None, ngroups=8):
    nc = bass.Bass("TRN2", target_bir_lowering=False)

    def din(name, shape, dt=F32):
        return nc.dram_tensor(name, list(shape), dt, kind="ExternalInput").ap()

    x_d = din("x", [S, D])
    conv_in_d = din("conv_in", [D, 3 * D])
    conv_out_d = din("conv_out", [D, D])
    qkv_d = din("attn_qkv", [D, 1536])
    wo_d = din("attn_out", [D, D])
    wg_d = din("w_gate", [2, NE, D, FF])
    wu_d = din("w_up", [2, NE, D, FF])
    wd_d = din("w_down", [2, NE, FF, D])
    gm_d = din("gm", [128, 2, 8])
    gf_d = din("gf", [128, 2, 8])
    gfin_d = din("gfin", [128, D])
    cw_d = din("cw", [128, 8, 3])
    rt_d = din("rt", [128, 2, 8, NE])
    gq_d = din("gq", [128, 64])
    gk_d = din("gk", [128, 64])
    tab_d = din("tab", [128, NT, 2, 2, 16])
    id32_d = din("id32", [128, 128])
    idbf_d = din("idbf", [128, 128], BF16)
    iota1_d = din("iota1", [128, CAP])
    pj_d = din("pj", [128, NT, NE, 2], BF16)
    gmat_d = din("gmat", [128, 128])
    lmat_d = din("lmat", [128, 128])

    okind = "ExternalOutput"
    out_d = nc.dram_tensor("out", [S, D], F32, kind=okind).ap()
    ikind = "ExternalOutput" if debug else "Internal"
    hA_d = nc.dram_tensor("hA", [S, D], F32, kind=ikind).ap()
    hB_d = nc.dram_tensor("hB", [S, D], F32, kind=ikind).ap()
    xn2_d = nc.dram_tensor("xn2", [S, D], BF16, kind=ikind).ap()
    qs_d = nc.dram_tensor("qscr", [S, D], BF16, kind=ikind).ap()
    if debug:
        dbg_log = nc.dram_tensor("dbg_log", [128, NT, NE], F32, kind=okind).ap()
        dbg_idx = nc.dram_tensor("dbg_idx", [128, NE, 4], I32, kind=okind).ap()
        dbg_gate = nc.dram_tensor("dbg_gate", [128, NE, 4], F32, kind=okind).ap()

    with ExitStack() as top:
        P = Prog(nc, top)

        def mm(out, lhsT, rhs, start, stop, R, W):
            P.op("pe", lambda E: E.matmul(out, lhsT, rhs, start=start, stop=stop), R, W)

        def tr(out, in_, ident, R, W):
            P.op("pe", lambda E: E.transpose(out, in_, ident), R, W)

        def act(out, in_, func, R, W, scale=None, bias=None, accum=None):
            kw = {}
            if scale is not None:
                kw["scale"] = scale
            if bias is not None:
                kw["bias"] = bias
            if accum is not None:
                kw["accum_out"] = accum
            P.op("act", lambda E: E.activation(out, in_, func, **kw), R, W)

        def ts(eng, out, in0, s1, s2, op0, op1, R, W):
            if op1 is None:
                P.op(eng, lambda E: E.tensor_scalar(out, in0, s1, None, op0), R, W)
            else:
                P.op(eng, lambda E: E.tensor_scalar(out, in0, s1, s2, op0, op1), R, W)

        def tt(eng, out, in0, in1, op, R, W):
            P.op(eng, lambda E: E.tensor_tensor(out, in0, in1, op), R, W)

        def stt(out, in0, scalar, in1, op0, op1, R, W):
            P.op("dve", lambda E: E.scalar_tensor_tensor(out, in0, scalar, in1, op0, op1), R, W)

        def dma(q, out, in_, R, W, owner=None, **kw):
            P.dma(q, lambda E: E.dma_start(out=out, in_=in_, **kw), R, W, owner=owner)

        gm = P.sb(top, "gm", [128, 2, 8], F32)
        gf = P.sb(top, "gf", [128, 2, 8], F32)
        cw = P.sb(top, "cw", [128, 8, 3], F32)
        rt = P.sb(top, "rt", [128, 2, 8, NE], F32)
        id32 = P.sb(top, "id32", [128, 128], F32)
        idbf = P.sb(top, "idbf", [128, 128], BF16)
        logits = P.sb(top, "logits", [128, NT, NE], F32)
        for tl, src in ((gm, gm_d), (gf, gf_d), (cw, cw_d), (rt, rt_d), (id32, id32_d), (idbf, idbf_d)):
            dma("sp", tl.t[:], src, (), (tl,))

        banks = [Tile(top.enter_context(nc.psum_tensor("bank%d" % i, [128, 512], F32)), "bank%d" % i)
                 for i in range(8)]

        def bk_bf(i):
            return banks[i].t[:, :].bitcast(BF16)

        x_b = Buf("x")
        sc_bufs = [Buf("sc0"), Buf("sc1")]
        hA_b = [Buf("hA0"), Buf("hA1")]
        hB_b = [Buf("hB0"), Buf("hB1")]
        xn2_b = Buf("xn2")
        qs_b = Buf("qscr")
        out_b = Buf("out")

        def rstd_from_ss(st_tiles, ss, n, scale_extra=1.0, inv_dim=1.0 / D):
            ms, sd, rs = st_tiles
            ts("dve", ms.t[0:n, :], ss.t[0:n, :], inv_dim, EPS, ALU.mult, ALU.add, (ss,), (ms,))
            act(sd.t[0:n, :], ms.t[0:n, :], AF.Sqrt, (ms,), (sd,))
            P.op("dve", lambda E: E.reciprocal(rs.t[0:n, :], sd.t[0:n, :]), (sd,), (rs,))
            return rs

        def post_mixer(pm, t, po_banks, res_src, res_bufs, dst, dst_bufs, layer, tb0, tb1, r_pre=None):
            rows = slice(t * 128, (t + 1) * 128)
            if r_pre is not None:
                r = r_pre
            else:
                r = pm["r"].next()
                dma("sp", r.t[:], res_src[rows, :], tuple(res_bufs), (r,))
            hn = pm["hn"].next()
            for hf in range(2):
                cs = slice(hf * 512, (hf + 1) * 512)
                tt("dve", hn.t[:, cs], po_banks[hf].t[:, :], r.t[:, cs], ALU.add, (po_banks[hf], r), (hn,))
            P.dma("sp", lambda E: E.dma_start(out=dst[rows, :], in_=hn.t[:]), (hn,), (), nodep_writes=tuple(dst_bufs), owner=hn)
            junk = pm["junk"]
            ss = pm["ss"].next()
            act(junk.t[:], hn.t[:], AF.Square, (hn,), (junk, ss), accum=ss.t[:, :])
            rs = rstd_from_ss(pm["st"].next(), ss, 128)
            xo = pm["xo"].next()
            act(xo.t[:], hn.t[:], AF.Copy, (hn, rs), (xo,), scale=rs.t[:, 0:1])
            P.dma("sp", lambda E: E.dma_start(out=xn2_d[rows, :], in_=xo.t[:]), (xo,), (), nodep_writes=(xn2_b,), owner=xo)
            hT = pm["hT"].next()
            for hf in range(2):
                tb = (tb0, tb1)[hf]
                for kk in range(4):
                    k = hf * 4 + kk
                    tr(tb.t[:, kk * 128:(kk + 1) * 128], hn.t[:, k * 128:(k + 1) * 128], id32.t[:], (hn, id32), (tb,))
                tt("dve", hT.t[:, hf * 4:(hf + 1) * 4, :],
                   tb.t[:, :].rearrange("p (k c) -> p k c", k=4),
                   gf.t[:, layer, hf * 4:(hf + 1) * 4].unsqueeze(2).to_broadcast([128, 4, 128]),
                   ALU.mult, (tb, gf), (hT,))
            for k in range(8):
                mm(tb0.t[:, 0:NE], hT.t[:, k, :], rt.t[:, layer, k, :], k == 0, k == 7, (hT, rt), (tb0,))
            ts("dve", logits.t[:, t, :], tb0.t[:, 0:NE], rs.t[:, 0:1], None, ALU.mult, None, (tb0, rs), (logits,))

        def alloc_post_mixer(st):
            pm = {}
            pm["r"] = Ring([P.sb(st, "pm_r", [128, D], F32) for _ in range(4)])
            pm["hn"] = Ring([P.sb(st, "pm_hn", [128, D], F32) for _ in range(3)])
            pm["junk"] = P.sb(st, "pm_junk", [128, D], BF16)
            pm["ss"] = Ring([P.sb(st, "pm_ss", [128, 1], F32) for _ in range(2)])
            pm["st"] = Ring([[P.sb(st, "pm_st", [128, 1], F32) for _ in range(3)] for _ in range(8)])
            pm["xo"] = Ring([P.sb(st, "pm_xo", [128, D], BF16) for _ in range(2)])
            pm["hT"] = Ring([P.sb(st, "pm_hT", [128, 8, 128], F32) for _ in range(3)])
            return pm

        def phase_conv():
            with ExitStack() as st:
                wp = [P.sb(st, "cwp", [128, 8, 1024], BF16) for _ in range(4)]
                for i in range(3):
                    dma("pool", wp[i].t[:], conv_in_d[:, i * 1024:(i + 1) * 1024].rearrange("(k p) f -> p k f", p=128), (), (wp[i],))
                dma("pool", wp[3].t[:], conv_out_d.rearrange("(k p) f -> p k f", p=128), (), (wp[3],))
                Wb, Wc, Wh, Wo = wp
                pm = alloc_post_mixer(st)
                xts = Ring([P.sb(st, "xt", [128, D], F32) for _ in range(4)])
                xns = Ring([P.sb(st, "xn", [128, D], BF16) for _ in range(3)])
                junk = P.sb(st, "cjunk", [128, D], BF16)
                sss = Ring([P.sb(st, "css", [128, 1], F32) for _ in range(2)])
                sts = Ring([[P.sb(st, "cst", [128, 1], F32) for _ in range(3)] for _ in range(2)])
                xnTs = [P.sb(st, "xnT", [128, 8, 1026], BF16)] * 2
                zTs = [P.sb(st, "zT", [128, 8, 1024], BF16)] * 2
                us = Ring([P.sb(st, "u", [128, 1026], F32) for _ in range(2)])
                ctmp = Ring([P.sb(st, "ctmp", [128, 512], F32) for _ in range(2)])
                ys = Ring([P.sb(st, "y", [128, 512], F32) for _ in range(2)])
                trb = Ring([0, 1])
                pcb = Ring([2, 3])
                phb = Ring([4, 5])
                pbb = Ring([6, 7])

                def norm_gen(xnT, r0, n, col0):
                    xt = xts.next()
                    dma("sp", xt.t[0:n, :], x_d[r0:r0 + n, :], (x_b,), (xt,))
                    yield
                    ss = sss.next()
                    act(junk.t[0:n, :], xt.t[0:n, :], AF.Square, (xt,), (junk, ss), accum=ss.t[0:n, :])
                    rs = rstd_from_ss(sts.next(), ss, n)
                    xn = xns.next()
                    ts("dve", xn.t[0:n, :], xt.t[0:n, :], rs.t[0:n, 0:1], None, ALU.mult, None, (xt, rs), (xn,))
                    yield
                    bi = trb.next()
                    pv = bk_bf(bi)
                    for k in range(8):
                        tr(pv[:, k * 128:k * 128 + n], xn.t[0:n, k * 128:(k + 1) * 128], idbf.t[0:n, 0:n], (xn, idbf), (banks[bi],))
                    yield
                    tt("dve", xnT.t[:, :, col0:col0 + n],
                       pv.rearrange("p (k c) -> p k c", k=8)[:, :, 0:n],
                       gm.t[:, 0, :].unsqueeze(2).to_broadcast([128, 8, n]),
                       ALU.mult, (banks[bi], gm), (xnT,))

                def run_skewed(gens_, offs):
                    m = len(gens_)
                    nst = len(offs)
                    for sl in range(m + offs[-1]):
                        for k in reversed(range(nst)):
                            i = sl - offs[k]
                            if 0 <= i < m:
                                try:
                                    next(gens_[i])
                                except StopIteration:
                                    pass

                def norm_rows_all(specs):
                    run_skewed([norm_gen(*sp) for sp in specs], [0, 2, 3, 4])

                def norm_specs(qi):
                    lo = qi * 1024
                    hi = lo + 1024
                    xq = xnTs[qi % 2]
                    specs = []
                    if lo > 0:
                        specs.append((xq, lo - 1, 1, 0))
                    for tl in range(8):
                        specs.append((xq, lo + tl * 128, 128, 1 + tl * 128))
                    if hi < S:
                        specs.append((xq, hi, 1, 1025))
                    return specs

                def channel_j(qi, j):
                    lo = qi * 1024
                    hi = lo + 1024
                    xnT = xnTs[qi % 2]
                    zT = zTs[qi % 2]
                    c_lo = 0 if lo > 0 else 1
                    c_hi = 1026 if hi < S else 1025
                    groups = []
                    c = c_lo
                    while c < c_hi:
                        n = min(512, c_hi - c)
                        groups.append((c, n))
                        c += n
                    js = slice(j * 128, (j + 1) * 128)
                    u = us.next()
                    if c_lo == 1:
                        P.op("pool", lambda E, u=u: E.memset(u.t[:, 0:1], 0.0), (), (u,))
                    if c_hi == 1025:
                        P.op("pool", lambda E, u=u: E.memset(u.t[:, 1025:1026], 0.0), (), (u,))
                    for (c0, n) in groups:
                        pc = banks[pcb.next()]
                        ph = banks[phb.next()]
                        for k in range(8):
                            mm(pc.t[:, 0:n], Wc.t[:, k, js], xnT.t[:, k, c0:c0 + n], k == 0, k == 7, (Wc, xnT), (pc,))
                        for k in range(8):
                            mm(ph.t[:, 0:n], Wh.t[:, k, js], xnT.t[:, k, c0:c0 + n], k == 0, k == 7, (Wh, xnT), (ph,))
                        ct = ctmp.next()
                        act(ct.t[:, 0:n], pc.t[:, 0:n], AF.Copy, (pc,), (ct,))
                        tt("dve", u.t[:, c0:c0 + n], ct.t[:, 0:n], ph.t[:, 0:n], ALU.mult, (ct, ph), (u,))
                    for g in range(2):
                        pb = banks[pbb.next()]
                        for k in range(8):
                            mm(pb.t[:, :], Wb.t[:, k, js], xnT.t[:, k, 1 + g * 512:1 + (g + 1) * 512], k == 0, k == 7, (Wb, xnT), (pb,))
                        y = ys.next()
                        a = g * 512
                        ts("dve", y.t[:], u.t[:, a:a + 512], cw.t[:, j, 0:1], None, ALU.mult, None, (u, cw), (y,))
                        stt(y.t[:], u.t[:, a + 1:a + 513], cw.t[:, j, 1:2], y.t[:], ALU.mult, ALU.add, (u, cw, y), (y,))
                        stt(y.t[:], u.t[:, a + 2:a + 514], cw.t[:, j, 2:3], y.t[:], ALU.mult, ALU.add, (u, cw, y), (y,))
                        tt("dve", zT.t[:, j, a:a + 512], y.t[:], pb.t[:, :], ALU.mult, (y, pb), (zT,))

                lgb = Ring([6, 7])

                def post_gen(qi, tl):
                    zT = zTs[qi % 2]
                    t = qi * 8 + tl
                    rows = slice(t * 128, (t + 1) * 128)
                    r = pm["r"].next()
                    dma("sp", r.t[:], x_d[rows, :], (x_b,), (r,))
                    yield
                    pob = (banks[2 + (tl % 2) * 2], banks[3 + (tl % 2) * 2])
                    for hf in range(2):
                        for k in range(8):
                            mm(pob[hf].t[:, :], zT.t[:, k, tl * 128:(tl + 1) * 128], Wo.t[:, k, hf * 512:(hf + 1) * 512],
                               k == 0, k == 7, (zT, Wo), (pob[hf],))
                    yield
                    hn = pm["hn"].next()
                    for hf in range(2):
                        cs = slice(hf * 512, (hf + 1) * 512)
                        tt("dve", hn.t[:, cs], pob[hf].t[:, :], r.t[:, cs], ALU.add, (pob[hf], r), (hn,))
                    P.dma("sp", lambda E: E.dma_start(out=hA_d[rows, :], in_=hn.t[:]), (hn,), (), nodep_writes=tuple(hA_b), owner=hn)
                    pjunk = pm["junk"]
                    ss = pm["ss"].next()
                    act(pjunk.t[:], hn.t[:], AF.Square, (hn,), (pjunk, ss), accum=ss.t[:, :])
                    rs = rstd_from_ss(pm["st"].next(), ss, 128)
                    xo = pm["xo"].next()
                    act(xo.t[:], hn.t[:], AF.Copy, (hn, rs), (xo,), scale=rs.t[:, 0:1])
                    P.dma("sp", lambda E: E.dma_start(out=xn2_d[rows, :], in_=xo.t[:]), (xo,), (), nodep_writes=(xn2_b,), owner=xo)
                    yield
                    tbs = (banks[0], banks[1])
                    for hf in range(2):
                        for kk in range(4):
                            k = hf * 4 + kk
                            tr(tbs[hf].t[:, kk * 128:(kk + 1) * 128], hn.t[:, k * 128:(k + 1) * 128], id32.t[:], (hn, id32), (tbs[hf],))
                    yield
                    hT = pm["hT"].next()
                    for hf in range(2):
                        tt("dve", hT.t[:, hf * 4:(hf + 1) * 4, :], tbs[hf].t[:, :].rearrange("p (k c) -> p k c", k=4),
                           gf.t[:, 0, hf * 4:(hf + 1) * 4].unsqueeze(2).to_broadcast([128, 4, 128]), ALU.mult, (tbs[hf], gf), (hT,))
                    yield
                    lb = banks[lgb.next()]
                    for k in range(8):
                        mm(lb.t[:, 0:NE], hT.t[:, k, :], rt.t[:, 0, k, :], k == 0, k == 7, (hT, rt), (lb,))
                    yield
                    ts("dve", logits.t[:, t, :], lb.t[:, 0:NE], rs.t[:, 0:1], None, ALU.mult, None, (lb, rs), (logits,))

                def post_steps_q(qi):
                    return [lambda: run_skewed([post_gen(qi, tl) for tl in range(8)], [0, 2, 3, 4, 5, 6, 7])]

                for qi in range(4):
                    norm_rows_all(norm_specs(qi))
                    for j in range(8):
                        channel_j(qi, j)
                    for st_ in post_steps_q(qi):
                        st_()
                P.emit()

        def phase_moe(layer, h_d, h_bufs):
            with ExitStack() as st:
                NR = 8
                ring = Ring([P.sb(st, "wr", [128, 8, 1024], BF16) for _ in range(NR)])
                idx = P.sb(st, "idx", [128, NE, 4], I32)
                gates = P.sb(st, "gates", [128, NE, 4], F32)
                pmk = P.sb(st, "pmk", [128, NT, NE], F32)
                tv = P.sb(st, "tv", [128, NT, NE, 5], BF16)
                iota1 = P.sb(st, "iota1", [128, CAP], F32)
                ohs = Ring([P.sb(st, "oh", [128, CAP], BF16) for _ in range(4)])
                idxT = P.sb(st, "idxT", [8, CAP], F32)
                idxf = P.sb(st, "idxf", [128, 4, 5], F32)
                IB = 7

                def oh_dve(e, j):
                    oh = ohs.next()
                    ts("dve", oh.t[:], iota1.t[:], pmk.t[:, j, e:e + 1], None, ALU.is_equal, None, (iota1, pmk), (oh,))
                    return oh

                def oh_pe(e, j, oh):
                    mm(banks[IB].t[0:5, :], tv.t[:, j, e, :], oh.t[:], j == 0, j == NT - 1, (oh, tv), (banks[IB],))

                def oh_finish(e):
                    P.op("dve", lambda E: E.tensor_copy(idxT.t[0:5, :], banks[IB].t[0:5, :]), (banks[IB],), (idxT,))
                    for c in range(4):
                        tr(banks[IB].t[:, c * 8:c * 8 + 5], idxT.t[0:5, c * 128:(c + 1) * 128], id32.t[0:5, 0:5], (idxT, id32), (banks[IB],))
                    P.op("dve", lambda E: E.tensor_copy(idxf.t[:], banks[IB].t[:, 0:32].rearrange("p (c f) -> p c f", c=4)[:, :, 0:5]),
                         (banks[IB],), (idxf,))
                    stt(idx.t[:, e, :], idxf.t[:, :, 1], 128.0, idxf.t[:, :, 0], ALU.mult, ALU.add, (idxf,), (idx,))
                    P.op("dve", lambda E, e=e: E.tensor_reduce(gates.t[:, e, :], idxf.t[:, :, 2:5], AX.X, ALU.add), (idxf,), (gates,))

                def load_piece(src):
                    w = ring.next()
                    dma("pool", w.t[:], src, (), (w,))
                    return w

                def expert_pieces(e, which):
                    if which == "g":
                        return [load_piece(wg_d[layer, e][:, hf * 1024:(hf + 1) * 1024].rearrange("(k p) f -> p k f", p=128)) for hf in range(2)]
                    if which == "u":
                        return [load_piece(wu_d[layer, e][:, hf * 1024:(hf + 1) * 1024].rearrange("(k p) f -> p k f", p=128)) for hf in range(2)]
                    return [load_piece(wd_d[layer, e][hf * 1024:(hf + 1) * 1024, :].rearrange("(k p) f -> p k f", p=128)) for hf in range(2)]

                def half(wd_, e, hf):
                    return wd_[layer, e][:, hf * 1024:(hf + 1) * 1024].rearrange("(k p) f -> p k f", p=128)
                g0_ = load_piece(half(wg_d, 0, 0))
                u0_ = load_piece(half(wu_d, 0, 0))
                g1_ = load_piece(half(wg_d, 0, 1))
                u1_ = load_piece(half(wu_d, 0, 1))
                w_d0 = expert_pieces(0, "d")
                cur_w = {"g": [g0_, g1_], "u": [u0_, u1_], "d": w_d0}

                with ExitStack() as rs_:
                    ex = P.sb(rs_, "ex", [128, NT, NE], F32)
                    aff = P.sb(rs_, "aff", [128, NT, NE], F32)
                    sm = P.sb(rs_, "sm", [128, NT], F32)
                    rsm = P.sb(rs_, "rsm", [128, NT], F32)
                    affS = P.sb(rs_, "affS", [128, 512], F32)
                    maskS = P.sb(rs_, "maskS", [128, 512], F32)
                    cjunk = P.sb(rs_, "cjunk", [128, 512], BF16)
                    mid = P.sb(rs_, "mid", [128, 1], F32)
                    thr = P.sb(rs_, "thr", [128, 1], F32)
                    cnt = P.sb(rs_, "cnt", [128, 1], F32)
                    tmpc = P.sb(rs_, "tmpc", [128, 1], F32)
                    one128 = P.sb(rs_, "one128", [128, 1], F32)
                    gmat = P.sb(rs_, "gmat", [128, 128], F32)
                    lmat = P.sb(rs_, "lmat", [128, 128], F32)
                    dma("sp", gmat.t[:], gmat_d, (), (gmat,))
                    dma("sp", lmat.t[:], lmat_d, (), (lmat,))
                    r1 = P.sb(rs_, "r1", [128, NT, NE], F32)
                    hi32 = P.sb(rs_, "hi32", [128, NT, NE], F32)

                    dma("sp", iota1.t[:], iota1_d, (), (iota1,))
                    pjc = P.sb(rs_, "pjc", [128, NT, NE, 2], BF16)
                    dma("sp", pjc.t[:], pj_d, (), (pjc,))
                    P.op("dve", lambda E: E.tensor_copy(tv.t[:, :, :, 0:2], pjc.t[:]), (pjc,), (tv,))
                    P.op("pool", lambda E: E.memset(one128.t[:], 1.0), (), (one128,))
                    act(ex.t[:], logits.t[:], AF.Exp, (logits,), (ex,))
                    P.op("dve", lambda E: E.tensor_reduce(sm.t[:], ex.t[:], AX.X, ALU.add), (ex,), (sm,))
                    P.op("dve", lambda E: E.reciprocal(rsm.t[:], sm.t[:]), (sm,), (rsm,))
                    tt("dve", aff.t[:], ex.t[:], rsm.t[:, :].unsqueeze(2).to_broadcast([128, NT, NE]), ALU.mult, (ex, rsm), (aff,))
                    P.op("dve", lambda E: E.tensor_copy(tv.t[:, :, :, 2], aff.t[:]), (aff,), (tv,))
                    P.op("dve", lambda E: E.tensor_copy(hi32.t[:], tv.t[:, :, :, 2]), (tv,), (hi32,))
                    tt("dve", r1.t[:], aff.t[:], hi32.t[:], ALU.subtract, (aff, hi32), (r1,))
                    P.op("dve", lambda E: E.tensor_copy(tv.t[:, :, :, 3], r1.t[:]), (r1,), (tv,))
                    P.op("dve", lambda E: E.tensor_copy(hi32.t[:], tv.t[:, :, :, 3]), (tv,), (hi32,))
                    tt("dve", r1.t[:], r1.t[:], hi32.t[:], ALU.subtract, (r1, hi32), (r1,))
                    P.op("dve", lambda E: E.tensor_copy(tv.t[:, :, :, 4], r1.t[:]), (r1,), (tv,))
                    tb_ = banks[0]
                    for b4 in range(4):
                        tr(tb_.t[:, b4 * 128:(b4 + 1) * 128], aff.t[:, b4 * 8:(b4 + 1) * 8, :].rearrange("p j e -> p (j e)"), id32.t[:],
                           (aff, id32), (tb_,))
                    act(affS.t[:], tb_.t[:, :], AF.Copy, (tb_,), (affS,))
                    P.op("pool", lambda E: E.memset(mid.t[:], 0.5), (), (mid,))
                    NIT = 24
                    for kq in range(NIT):
                        w = 2.0 ** -(kq + 1)
                        P.op("dve", lambda E: E.tensor_scalar(cjunk.t[:], affS.t[:], mid.t[:, 0:1], None, ALU.is_ge, ALU.add, accum_out=cnt.t[:, 0:1]),
                             (affS, mid), (cjunk, cnt))
                        mm(banks[1].t[:, 0:1], gmat.t[:], cnt.t[:, 0:1], True, True, (gmat, cnt), (banks[1],))
                        ts("dve", tmpc.t[:], banks[1].t[:, 0:1], 511.5, w, ALU.is_ge, ALU.mult, (banks[1],), (tmpc,))
                        stt(mid.t[:], tmpc.t[:], -w / 2, mid.t[:], ALU.add, ALU.add, (tmpc, mid), (mid,))
                    ts("dve", thr.t[:], mid.t[:], -(2.0 ** -(NIT + 1)), None, ALU.add, None, (mid,), (thr,))
                    P.op("dve", lambda E: E.tensor_scalar(maskS.t[:], affS.t[:], thr.t[:, 0:1], None, ALU.is_ge, ALU.add, accum_out=cnt.t[:, 0:1]),
                         (affS, thr), (maskS, cnt))
                    P.op("dve", lambda E: E.tensor_tensor_scan(out=affS.t[:], data0=one128.t[:, 0:1].to_broadcast([128, 512]), data1=maskS.t[:],
                                                              initial=0.0, op0=ALU.mult, op1=ALU.add), (one128, maskS), (affS,))
                    mm(banks[1].t[:, 0:1], lmat.t[:], cnt.t[:, 0:1], True, True, (lmat, cnt), (banks[1],))
                    P.op("dve", lambda E: E.tensor_copy(tmpc.t[:], banks[1].t[:, 0:1]), (banks[1],), (tmpc,))
                    stt(affS.t[:], affS.t[:], tmpc.t[:, 0:1], maskS.t[:], ALU.add, ALU.mult, (affS, tmpc, maskS), (affS,))
                    pmb = banks[0]
                    for b4 in range(4):
                        tr(pmb.t[:, b4 * 128:(b4 + 1) * 128], affS.t[:, b4 * 128:(b4 + 1) * 128], id32.t[:], (affS, id32), (pmb,))
                    P.op("dve", lambda E: E.tensor_copy(pmk.t[:], pmb.t[:, :].rearrange("p (j e) -> p j e", j=NT)), (pmb,), (pmk,))
                    for e in range(2 if stage >= 3 else NE):
                        for j in range(NT):
                            oh_pe(e, j, oh_dve(e, j))
                        oh_finish(e)
                    if debug and stage < 3:
                        dma("sp", dbg_idx, idx.t[:], (idx,), (Buf("dbgi"),))
                        dma("sp", dbg_gate, gates.t[:], (gates,), (Buf("dbgg"),))
                        dma("sp", dbg_log, logits.t[:], (logits,), (Buf("dbgl"),))
                    P.emit()

                if stage < 3:
                    P.emit()
                    return
                with ExitStack() as es_:
                    xgs = Ring([P.sb(es_, "xg", [128, 4, D], BF16) for _ in range(2)])
                    xgTs = Ring([P.sb(es_, "xgT", [128, 8, CAP], BF16) for _ in range(2)])
                    hT = P.sb(es_, "hTe", [128, 16, CAP], BF16)
                    sgs = Ring([P.sb(es_, "sg", [128, CAP], F32) for _ in range(2)])
                    ygs = Ring([P.sb(es_, "yg", [128, D], F32) for _ in range(2)])
                    pgb = Ring([0, 1])
                    pub = Ring([2, 3])
                    pyb = Ring([4, 5])
                    ptb = Ring([6])

                    def gather(e):
                        xg = xgs.next()
                        for c in range(4):
                            P.dma("pool", lambda E, c=c, xg=xg: E.indirect_dma_start(
                                out=xg.t[:, c, :], out_offset=None, in_=xn2_d[:, :],
                                in_offset=bass.IndirectOffsetOnAxis(ap=idx.t[:, e, c:c + 1], axis=0)),
                                (idx, xn2_b), (xg,) if c == 0 else (), nodep_writes=() if c == 0 else (xg,))
                        return xg

                    def transposes(xg):
                        xgT = xgTs.next()
                        for k in range(8):
                            bi = ptb.next()
                            pv = bk_bf(bi)
                            for c in range(4):
                                tr(pv[:, c * 128:(c + 1) * 128], xg.t[:, c, k * 128:(k + 1) * 128], idbf.t[:], (xg, idbf), (banks[bi],))
                            if k % 2 == 0:
                                ts("dve", xgT.t[:, k, :], pv[:, 0:CAP], gf.t[:, layer, k:k + 1], None, ALU.mult, None, (banks[bi], gf), (xgT,))
                            else:
                                act(xgT.t[:, k, :], pv[:, 0:CAP], AF.Copy, (banks[bi], gf), (xgT,), scale=gf.t[:, layer, k:k + 1])
                        return xgT

                    xg_cur = gather(0)
                    xgT_cur = transposes(xg_cur)
                    xg_next = gather(1)
                    sc_prev = h_bufs
                    for e in range(NE):
                        nxt = {}
                        if e + 1 < NE:
                            nxt["g"] = [load_piece(wg_d[layer, e + 1][:, 0:1024].rearrange("(k p) f -> p k f", p=128)), None]
                            nxt["u"] = [load_piece(wu_d[layer, e + 1][:, 0:1024].rearrange("(k p) f -> p k f", p=128)), None]
                        for fc in range(16):
                            ohl = [oh_dve(e + 2, 2 * fc), oh_dve(e + 2, 2 * fc + 1)] if e + 2 < NE else []
                            wgp = cur_w["g"][fc // 8]
                            wup = cur_w["u"][fc // 8]
                            fs = slice((fc % 8) * 128, (fc % 8 + 1) * 128)
                            pg = banks[pgb.next()]
                            pu = banks[pub.next()]
                            for k in range(8):
                                mm(pg.t[:, :], wgp.t[:, k, fs], xgT_cur.t[:, k, :], k == 0, k == 7, (wgp, xgT_cur), (pg,))
                            for k in range(8):
                                mm(pu.t[:, :], wup.t[:, k, fs], xgT_cur.t[:, k, :], k == 0, k == 7, (wup, xgT_cur), (pu,))
                            sg = sgs.next()
                            act(sg.t[:], pg.t[:, :], AF.Silu, (pg,), (sg,))
                            tt("dve", hT.t[:, fc, :], sg.t[:], pu.t[:, :], ALU.mult, (sg, pu), (hT,))
                            for jj_, oh_ in enumerate(ohl):
                                oh_pe(e + 2, 2 * fc + jj_, oh_)
                            if e + 1 < NE and fc == 7:
                                nxt["g"][1] = load_piece(wg_d[layer, e + 1][:, 1024:2048].rearrange("(k p) f -> p k f", p=128))
                                nxt["u"][1] = load_piece(wu_d[layer, e + 1][:, 1024:2048].rearrange("(k p) f -> p k f", p=128))
                        if e + 1 < NE:
                            nxt["d"] = expert_pieces(e + 1, "d")
                            xgT_next = transposes(xg_next)
                            if e + 2 < NE:
                                oh_finish(e + 2)
                                xg_next = gather(e + 2)
                        sc_cur = [sc_bufs[e % 2]]
                        for c in range(4):
                            yg = ygs.next()
                            for hf in range(2):
                                py = banks[pyb.next()]
                                for fc in range(16):
                                    wdp = cur_w["d"][fc // 8]
                                    mm(py.t[:, :], hT.t[:, fc, c * 128:(c + 1) * 128], wdp.t[:, fc % 8, hf * 512:(hf + 1) * 512],
                                       fc == 0, fc == 15, (hT, wdp), (py,))
                                if hf == 0:
                                    act(yg.t[:, 0:512], py.t[:, :], AF.Copy, (py, gates), (yg,), scale=gates.t[:, e, c:c + 1])
                                else:
                                    ts("dve", yg.t[:, 512:1024], py.t[:, :], gates.t[:, e, c:c + 1], None, ALU.mult, None, (py, gates), (yg,))
                            P.dma("pool", lambda E, c=c, yg=yg, e=e: E.indirect_dma_start(
                                out=h_d[:, :], out_offset=bass.IndirectOffsetOnAxis(ap=idx.t[:, e, c:c + 1], axis=0),
                                in_=yg.t[:], in_offset=None, compute_op=ALU.add),
                                [idx, yg] + list(sc_prev), (), nodep_writes=tuple(sc_cur), owner=yg)
                        sc_prev = sc_cur
                        if e + 1 < NE:
                            cur_w = nxt
                            xgT_cur = xgT_next
                    h_bufs[:] = list(h_bufs) + sc_prev
                    P.emit()

        def phase_attn():
            with ExitStack() as st:
                kTA = P.sb(st, "kTA", [128, 4, S], BF16)
                kTB = P.sb(st, "kTB", [128, 4, S], BF16)
                vx = P.sb(st, "vx", [128, NT, 4, 65], BF16)
                with ExitStack() as s1:
                    wq = P.sb(s1, "wq", [128, 8, 1024], BF16)
                    wkv = P.sb(s1, "wkv", [128, 8, 512], BF16)
                    dma("pool", wq.t[:], qkv_d[:, 0:1024].rearrange("(k p) f -> p k f", p=128), (), (wq,))
                    dma("pool", wkv.t[:], qkv_d[:, 1024:1536].rearrange("(k p) f -> p k f", p=128), (), (wkv,))
                    for k in range(8):
                        ts("dve", wq.t[:, k, :], wq.t[:, k, :], gm.t[:, 1, k:k + 1], None, ALU.mult, None, (wq, gm), (wq,))
                        act(wkv.t[:, k, :], wkv.t[:, k, :], AF.Copy, (wkv, gm), (wkv,), scale=gm.t[:, 1, k:k + 1])
                    tab = P.sb(s1, "tab", [128, NT, 2, 2, 16], F32)
                    gq = P.sb(s1, "gq", [128, 64], F32)
                    gk = P.sb(s1, "gk", [128, 64], F32)
                    dma("sp", tab.t[:], tab_d, (), (tab,))
                    dma("sp", gq.t[:], gq_d, (), (gq,))
                    dma("sp", gk.t[:], gk_d, (), (gk,))
                    P.op("pool", lambda E: E.memset(vx.t[:, :, :, 64:65], 1.0), (), (vx,))
                    xts = Ring([P.sb(s1, "axt", [128, D], F32) for _ in range(4)])
                    xns = Ring([P.sb(s1, "axn", [128, D], BF16) for _ in range(2)])
                    junk = P.sb(s1, "ajunk", [128, D], BF16)
                    sss = Ring([P.sb(s1, "ass", [128, 1], F32) for _ in range(2)])
                    sts = Ring([[P.sb(s1, "ast", [128, 1], F32) for _ in range(3)] for _ in range(4)])
                    xnTs = Ring([P.sb(s1, "axnT", [128, 8, 128], BF16) for _ in range(2)])
                    sq2 = [P.sb(s1, "sq", [128, 1280], F32) for _ in range(2)]
                    ssh2 = [P.sb(s1, "ssh", [128, 20], F32) for _ in range(2)]
                    hms2 = [P.sb(s1, "hms", [128, 20], F32) for _ in range(2)]
                    hsd2 = [P.sb(s1, "hsd", [128, 20], F32) for _ in range(2)]
                    hrs2 = [P.sb(s1, "hrs", [128, 20], F32) for _ in range(2)]
                    tq2 = [P.sb(s1, "tq", [128, 4, 2, 16], F32) for _ in range(2)]
                    tk2 = [P.sb(s1, "tk", [128, 4, 2, 16], F32) for _ in range(2)]
                    ab2 = [[P.sb(s1, "ab%d" % i, [128, 20, 2, 16], F32) for i in range(4)] for _ in range(2)]
                    rop2 = [P.sb(s1, "rop", [128, 20, 2, 2, 16], F32) for _ in range(2)]
                    qrs = Ring([P.sb(s1, "qr", [128, D], BF16) for _ in range(2)])
                    kds = Ring([P.sb(s1, "kd", [128, 2, 4, 2, 64], BF16) for _ in range(2)])
                    for kd_ in kds.items:
                        P.op(os.environ.get("KDENG", "pool"), lambda E, kd_=kd_: E.memset(kd_.t[:], 0.0), (), (kd_,))
                    mhalf1 = P.sb(s1, "mhalf1", [128, 1], F32)
                    P.op("pool", lambda E: E.memset(mhalf1.t[:], -0.5), (), (mhalf1,))
                    trb = Ring([0, 1])
                    pqb = Ring([(2, 3, 4), (5, 6, 7)])
                    gqv = gq.t[:, :].rearrange("p (a h f) -> p a h f", a=2, h=2)
                    gkv = gk.t[:, :].rearrange("p (a h f) -> p a h f", a=2, h=2)
                    def tile_ops(j):
                        rows = slice(j * 128, (j + 1) * 128)
                        xt = xts.next()
                        dma("sp", xt.t[:], hA_d[rows, :], tuple(hA_b), (xt,))
                        yield
                        ss = sss.next()
                        act(junk.t[:], xt.t[:], AF.Square, (xt,), (junk, ss), accum=ss.t[:, :])
                        ms, r2, rs = sts.next()
                        ts("dve", ms.t[:], ss.t[:], 1.0 / D, EPS, ALU.mult, ALU.add, (ss,), (ms,))
                        tt("pool", rs.t[:], ms.t[:], mhalf1.t[:], ALU.pow, (ms, mhalf1), (rs,))
                        stt(r2.t[:], rs.t[:], 1.0 / 64, rs.t[:], ALU.mult, ALU.mult, (rs,), (r2,))
                        xn = xns.next()
                        act(xn.t[:], xt.t[:], AF.Copy, (xt,), (xn,))
                        bi = trb.next()
                        pv = bk_bf(bi)
                        for k in range(8):
                            tr(pv[:, k * 128:(k + 1) * 128], xn.t[:, k * 128:(k + 1) * 128], idbf.t[:], (xn, idbf), (banks[bi],))
                        xnT = xnTs.next()
                        act(xnT.t[:], pv.rearrange("p (k c) -> p k c", k=8), AF.Copy, (banks[bi],), (xnT,))
                        yield
                        b3 = pqb.next()
                        for gi, (w, c0) in enumerate(((wq, 0), (wq, 512), (wkv, 0))):
                            pb = banks[b3[gi]]
                            for k in range(8):
                                mm(pb.t[:, :], xnT.t[:, k, :], w.t[:, k, c0:c0 + 512], k == 0, k == 7, (xnT, w), (pb,))
                        yield
                        rs_tok, r2_tok = rs, r2
                        jp = j % 2
                        sq, ssh, hms, hsd, hrs = sq2[jp], ssh2[jp], hms2[jp], hsd2[jp], hrs2[jp]
                        tq, tk, ab, rop = tq2[jp], tk2[jp], ab2[jp], rop2[jp]
                        pq0, pq1, pkv = banks[b3[0]], banks[b3[1]], banks[b3[2]]
                        act(vx.t[:, j, :, 0:64], pkv.t[:, 256:512].rearrange("p (h d) -> p h d", h=4), AF.Copy, (pkv, rs_tok), (vx,),
                            scale=rs_tok.t[:, 0:1])
                        act(sq.t[:, 0:512], pq0.t[:, :], AF.Square, (pq0,), (sq,))
                        act(sq.t[:, 512:1024], pq1.t[:, :], AF.Square, (pq1,), (sq,))
                        act(sq.t[:, 1024:1280], pkv.t[:, 0:256], AF.Square, (pkv,), (sq,))
                        P.op("dve", lambda E: E.tensor_reduce(ssh.t[:], sq.t[:, :].rearrange("p (h d) -> p h d", d=64), AX.X, ALU.add), (sq,), (ssh,))
                        ts("dve", hms.t[:], ssh.t[:], r2_tok.t[:, 0:1], EPS, ALU.mult, ALU.add, (ssh, r2_tok), (hms,))
                        act(hsd.t[:], hms.t[:], AF.Sqrt, (hms,), (hsd,))
                        P.op("dve", lambda E: E.reciprocal(hsd.t[:], hsd.t[:]), (hsd,), (hsd,))
                        ts("dve", hrs.t[:, 0:16], hsd.t[:, 0:16], rs_tok.t[:, 0:1], 0.125, ALU.mult, ALU.mult, (hsd, rs_tok), (hrs,))
                        ts("dve", hrs.t[:, 16:20], hsd.t[:, 16:20], rs_tok.t[:, 0:1], None, ALU.mult, None, (hsd, rs_tok), (hrs,))
                        cosv = tab.t[:, j, :, 0, :]
                        sinv = tab.t[:, j, :, 1, :]
                        for (tbl, gv) in ((tq, gqv), (tk, gkv)):
                            tt("pool", tbl.t[:, 0], cosv, gv[:, :, 0, :], ALU.mult, (tab, gq, gk), (tbl,))
                            tt("pool", tbl.t[:, 1], sinv, gv[:, :, 1, :], ALU.mult, (tab, gq, gk), (tbl,))
                            tt("pool", tbl.t[:, 2], cosv, gv[:, :, 1, :], ALU.mult, (tab, gq, gk), (tbl,))
                            tt("pool", tbl.t[:, 3], sinv, gv[:, :, 0, :], ALU.mult, (tab, gq, gk), (tbl,))
                        for (pb, h0, nh, tbl) in ((pq0, 0, 8, tq), (pq1, 8, 8, tq), (pkv, 16, 4, tk)):
                            v = pb.t[:, 0:nh * 64].rearrange("p (h a x f) -> p h a x f", h=nh, a=2, x=2)
                            x1 = v[:, :, :, 0, :]
                            x2 = v[:, :, :, 1, :]
                            hs = slice(h0, h0 + nh)

                            def bc(i):
                                return tbl.t[:, i].unsqueeze(1).to_broadcast([128, nh, 2, 16])
                            tt("dve", ab[0].t[:, hs], x1, bc(0), ALU.mult, (pb, tbl), (ab[0],))
                            tt("dve", ab[1].t[:, hs], x2, bc(1), ALU.mult, (pb, tbl), (ab[1],))
                            tt("dve", ab[2].t[:, hs], x2, bc(2), ALU.mult, (pb, tbl), (ab[2],))
                            tt("dve", ab[3].t[:, hs], x1, bc(3), ALU.mult, (pb, tbl), (ab[3],))
                        yield
                        tt("pool", rop.t[:, :, :, 0, :], ab[0].t[:], ab[1].t[:], ALU.subtract, (ab[0], ab[1]), (rop,))
                        tt("pool", rop.t[:, :, :, 1, :], ab[2].t[:], ab[3].t[:], ALU.add, (ab[2], ab[3]), (rop,))
                        qr = qrs.next()
                        kd = kds.next()
                        ropv = rop.t[:, :, :, :, :].rearrange("p h a x f -> p h (a x f)")
                        tt("dve", qr.t[:, :].rearrange("p (h d) -> p h d", d=64), ropv[:, 0:16, :],
                           hrs.t[:, 0:16].unsqueeze(2).to_broadcast([128, 16, 64]), ALU.mult, (rop, hrs), (qr,))
                        for ab_ in range(2):
                            tt("dve", kd.t[:, ab_, :, ab_, :], ropv[:, 16:20, :],
                               hrs.t[:, 16:20].unsqueeze(2).to_broadcast([128, 4, 64]), ALU.mult, (rop, hrs), (kd,))
                        P.dma("sp", lambda E, rows=rows, qr=qr: E.dma_start(out=qs_d[rows, :], in_=qr.t[:]), (qr,), (), nodep_writes=(qs_b,), owner=qr)
                        yield
                        bi = trb.next()
                        pv = bk_bf(bi)
                        for ab_ in range(2):
                            for h in range(4):
                                tr(pv[:, (ab_ * 4 + h) * 128:(ab_ * 4 + h + 1) * 128], kd.t[:, ab_, h, :, :].rearrange("p a d -> p (a d)"),
                                   idbf.t[:], (kd, idbf), (banks[bi],))
                        act(kTA.t[:, :, rows], pv[:, 0:512].rearrange("p (h c) -> p h c", h=4), AF.Copy, (banks[bi],), (kTA,))
                        act(kTB.t[:, :, rows], pv[:, 512:1024].rearrange("p (h c) -> p h c", h=4), AF.Copy, (banks[bi],), (kTB,))

                    gens = {}

                    def adv(t):
                        if 0 <= t < NT:
                            try:
                                next(gens[t])
                            except StopIteration:
                                pass
                    for t0_ in range(2):
                        gens[t0_] = tile_ops(t0_)
                        adv(t0_)
                    for sl in range(NT + 5):
                        if sl + 2 < NT:
                            gens[sl + 2] = tile_ops(sl + 2)
                            adv(sl + 2)
                        adv(sl - 4)
                        adv(sl - 2)
                        adv(sl - 3)
                        adv(sl - 1)
                        adv(sl)
                    P.emit()
                if stage < 5:
                    return
                with ExitStack() as s2:
                    wo = P.sb(s2, "wo", [128, 8, 1024], BF16)
                    dma("pool", wo.t[:], wo_d.rearrange("(k p) f -> p k f", p=128), (), (wo,))
                    pm = alloc_post_mixer(s2)
                    qgs = Ring([P.sb(s2, "qg", [128, 4, D], BF16) for _ in range(2)])
                    qTs = Ring([P.sb(s2, "qT", [128, 8, 512], BF16) for _ in range(2)])
                    pTs = Ring([P.sb(s2, "pT", [128, 512], BF16) for _ in range(4)])
                    aos = Ring([P.sb(s2, "ao", [128, 4, D], BF16) for _ in range(2)])
                    aoTs = Ring([P.sb(s2, "aoT", [128, 8, 128], BF16) for _ in range(2)])
                    rinvs = Ring([P.sb(s2, "rinv", [128, 1], F32) for _ in range(4)])
                    psb = Ring([0, 1, 6])

                    def load_q(g):
                        qg = qgs.next()
                        dma("sp", qg.t[:], qs_d[g * 512:(g + 1) * 512, :].rearrange("(c p) d -> p c d", p=128), (qs_b,), (qg,))
                        return qg

                    def q_transposes(qg):
                        qT = qTs.next()
                        for c in range(4):
                            bi = 7
                            pv = bk_bf(bi)
                            for pr in range(8):
                                tr(pv[:, pr * 128:(pr + 1) * 128], qg.t[:, c, pr * 128:(pr + 1) * 128], idbf.t[:], (qg, idbf), (banks[bi],))
                            P.op("dve", lambda E, c=c, pv=pv, qT=qT: E.tensor_copy(qT.t[:, :, c * 128:(c + 1) * 128], pv.rearrange("p (r c) -> p r c", r=8)),
                                 (banks[bi],), (qT,))
                        return qT

                    B7 = banks[7]
                    mhalf = P.sb(s2, "mhalf", [128, 1], F32)
                    P.op("pool", lambda E: E.memset(mhalf.t[:], -0.5), (), (mhalf,))

                    def q_steps(qg, qT):
                        steps = []
                        for c in range(4):
                            def step(c=c):
                                pv = bk_bf(7)
                                for pr in range(8):
                                    tr(pv[:, pr * 128:(pr + 1) * 128], qg.t[:, c, pr * 128:(pr + 1) * 128], idbf.t[:], (qg, idbf), (B7,))
                                P.op("dve", lambda E: E.tensor_copy(qT.t[:, :, c * 128:(c + 1) * 128], pv.rearrange("p (r c) -> p r c", r=8)),
                                     (B7,), (qT,))
                            steps.append(step)
                        return steps

                    def post_steps(g, ao):
                        steps = []
                        for qs in range(4):
                            t = g * 4 + qs
                            rows = slice(t * 128, (t + 1) * 128)
                            c = {}

                            def sA(qs=qs, rows=rows, c=c):
                                c["r"] = pm["r"].next()
                                dma("sp", c["r"].t[:], hA_d[rows, :], tuple(hA_b), (c["r"],))
                                pv = bk_bf(7)
                                for k in range(8):
                                    tr(pv[:, k * 128:(k + 1) * 128], ao.t[:, qs, k * 128:(k + 1) * 128], idbf.t[:], (ao, idbf), (B7,))
                                c["aoT"] = aoTs.next()
                                aoT = c["aoT"]
                                P.op("dve", lambda E: E.tensor_copy(aoT.t[:], pv.rearrange("p (k c) -> p k c", k=8)), (B7,), (aoT,))
                                c["hn"] = pm["hn"].next()

                            def sO(hf, rows=rows, c=c):
                                aoT, hn, r = c["aoT"], c["hn"], c["r"]
                                cs = slice(hf * 512, (hf + 1) * 512)
                                for k in range(8):
                                    mm(B7.t[:, :], aoT.t[:, k, :], wo.t[:, k, cs], k == 0, k == 7, (aoT, wo), (B7,))
                                tt("dve", hn.t[:, cs], B7.t[:, :], r.t[:, cs], ALU.add, (B7, r), (hn,))
                                if hf == 1:
                                    P.dma("sp", lambda E: E.dma_start(out=hB_d[rows, :], in_=hn.t[:]), (hn,), (), nodep_writes=tuple(hB_b), owner=hn)
                                    junk = pm["junk"]
                                    ss = pm["ss"].next()
                                    P.op("dve", lambda E: E.scalar_tensor_tensor(junk.t[:], hn.t[:], 1.0, hn.t[:], ALU.mult, ALU.mult, accum_out=ss.t[:, 0:1]),
                                         (hn,), (junk, ss))
                                    ms, _, rs = pm["st"].next()
                                    ts("dve", ms.t[:], ss.t[:], 1.0 / D, EPS, ALU.mult, ALU.add, (ss,), (ms,))
                                    tt("pool", rs.t[:], ms.t[:], mhalf.t[:], ALU.pow, (ms, mhalf), (rs,))
                                    c["rs"] = rs
                                    xo = pm["xo"].next()
                                    ts("dve", xo.t[:], hn.t[:], rs.t[:, 0:1], None, ALU.mult, None, (hn, rs), (xo,))
                                    P.dma("sp", lambda E: E.dma_start(out=xn2_d[rows, :], in_=xo.t[:]), (xo,), (), nodep_writes=(xn2_b,), owner=xo)
                                    c["hT"] = pm["hT"].next()

                            def sH(hf, c=c):
                                hn, hT = c["hn"], c["hT"]
                                for kk in range(4):
                                    k = hf * 4 + kk
                                    tr(B7.t[:, kk * 128:(kk + 1) * 128], hn.t[:, k * 128:(k + 1) * 128], id32.t[:], (hn, id32), (B7,))
                                tt("dve", hT.t[:, hf * 4:(hf + 1) * 4, :], B7.t[:, :].rearrange("p (k c) -> p k c", k=4),
                                   gf.t[:, 1, hf * 4:(hf + 1) * 4].unsqueeze(2).to_broadcast([128, 4, 128]), ALU.mult, (B7, gf), (hT,))

                            def sL(t=t, c=c):
                                hT, rs = c["hT"], c["rs"]
                                for k in range(8):
                                    mm(B7.t[:, 0:NE], hT.t[:, k, :], rt.t[:, 1, k, :], k == 0, k == 7, (hT, rt), (B7,))
                                ts("dve", logits.t[:, t, :], B7.t[:, 0:NE], rs.t[:, 0:1], None, ALU.mult, None, (B7, rs), (logits,))

                            steps += [sA, lambda sO=sO: sO(0), lambda sO=sO: sO(1), lambda sH=sH: sH(0), lambda sH=sH: sH(1), sL]
                        return steps

                    items = [(h, kt) for h in range(16) for kt in range(NT)]
                    qg0 = load_q(0)
                    qT_cur = qTs.next()
                    for st_ in q_steps(qg0, qT_cur):
                        st_()
                    pending = []
                    for g in range(ngroups):
                        qT = qT_cur
                        if g + 1 < ngroups:
                            qg_n = load_q(g + 1)
                            qT_next = qTs.next()
                            pending = pending + q_steps(qg_n, qT_next)
                        ao = aos.next()

                        def s_step(i):
                            h, kt = items[i]
                            pr, hf, kvh = h // 2, h % 2, h // 4
                            kTx = kTA if hf == 0 else kTB
                            psk = banks[psb.next()]
                            mm(psk.t[:, :], kTx.t[:, kvh, kt * 128:(kt + 1) * 128], qT.t[:, pr, :], True, True, (kTx, qT), (psk,))
                            pT = pTs.next()
                            act(pT.t[:], psk.t[:, :], AF.Exp, (psk,), (pT,))
                            return pT

                        LA = 2
                        pends = [s_step(i) for i in range(LA)]
                        for i in range(len(items)):
                            h, kt = items[i]
                            kvh = h // 4
                            if i + LA < len(items):
                                pends.append(s_step(i + LA))
                            pend = pends.pop(0)
                            for qs in range(4):
                                mm(banks[2 + qs].t[:, 0:65], pend.t[:, qs * 128:(qs + 1) * 128], vx.t[:, kt, kvh, :], kt == 0, kt == NT - 1,
                                   (pend, vx), (banks[2 + qs],))
                            if kt == NT - 1:
                                for qs in range(4):
                                    ri = rinvs.next()
                                    P.op("dve", lambda E, qs=qs, ri=ri: E.reciprocal(ri.t[:], banks[2 + qs].t[:, 64:65]), (banks[2 + qs],), (ri,))
                                    ts("dve", ao.t[:, qs, h * 64:(h + 1) * 64], banks[2 + qs].t[:, 0:64], ri.t[:, 0:1], None, ALU.mult, None,
                                       (banks[2 + qs], ri), (ao,))
                            if pending and i % 16 == 8:
                                pending.pop(0)()
                        while pending:
                            pending.pop(0)()
                        pending = post_steps(g, ao)
                        if g + 1 < ngroups:
                            qT_cur = qT_next
                    while pending:
                        pending.pop(0)()
                    P.emit()

        def phase_final(src_d, src_bufs):
            with ExitStack() as st:
                gfin = P.sb(st, "gfin", [128, D], F32)
                dma("sp", gfin.t[:], gfin_d, (), (gfin,))
                xts = Ring([P.sb(st, "fxt", [128, D], F32) for _ in range(4)])
                ys = Ring([P.sb(st, "fy", [128, D], F32) for _ in range(3)])
                junk = P.sb(st, "fjunk", [128, D], BF16)
                sss = Ring([P.sb(st, "fss", [128, 1], F32) for _ in range(2)])
                sts = Ring([[P.sb(st, "fst", [128, 1], F32) for _ in range(3)] for _ in range(2)])
                def fload(t):
                    xt_ = xts.next()
                    dma("sp", xt_.t[:], src_d[t * 128:(t + 1) * 128, :], tuple(src_bufs), (xt_,))
                    return xt_
                pre = [fload(0), fload(1)]
                for t in range(NT):
                    rows = slice(t * 128, (t + 1) * 128)
                    if t + 2 < NT:
                        pre.append(fload(t + 2))
                    xt = pre.pop(0)
                    ss = sss.next()
                    act(junk.t[:], xt.t[:], AF.Square, (xt,), (junk, ss), accum=ss.t[:, :])
                    rs = rstd_from_ss(sts.next(), ss, 128)
                    y = ys.next()
                    stt(y.t[:], xt.t[:], rs.t[:, 0:1], gfin.t[:], ALU.mult, ALU.mult, (xt, rs, gfin), (y,))
                    P.dma("sp", lambda E, rows=rows, y=y: E.dma_start(out=out_d[rows, :], in_=y.t[:]), (y,), (), nodep_writes=(out_b,), owner=y)
                P.wait_all("sp", [out_b])
                P.emit()

        if only is None or only == "conv":
            phase_conv()
        last_d, last_b = hA_d, hA_b
        if stage >= 2 and only is None:
            phase_moe(0, hA_d, hA_b)
        if stage >= 4:
            phase_attn()
        if stage >= 6 and only is None:
            phase_moe(1, hB_d, hB_b)
            last_d, last_b = hB_d, hB_b
        phase_final(last_d, last_b)
    return nc


def _constants():
    f32 = np.float32
    c = {}
    c["id32"] = np.eye(128, dtype=f32)
    c["idbf"] = np.eye(128, dtype=f32).astype(ml_dtypes.bfloat16)
    c["iota1"] = np.ascontiguousarray(np.broadcast_to(np.arange(1, CAP + 1, dtype=f32)[None, :], (128, CAP)))
    pj = np.zeros((128, NT, NE, 2), dtype=f32)
    pj[:, :, :, 0] = np.arange(128, dtype=f32)[:, None, None]
    pj[:, :, :, 1] = np.arange(NT, dtype=f32)[None, :, None]
    c["pj"] = pj.astype(ml_dtypes.bfloat16)
    pp = np.arange(128)
    same = (pp[:, None] % NE) == (pp[None, :] % NE)
    c["gmat"] = same.astype(f32)
    c["lmat"] = (same & ((pp[:, None] // NE) < (pp[None, :] // NE))).astype(f32)
    t = np.arange(S)
    row = (t // 64).astype(f32)
    col = (t % 64).astype(f32)
    inv_freq = (f32(10000.0) ** (-np.arange(0, 32, 2, dtype=f32) / f32(32))).astype(f32)
    ang_r = (row[:, None] * inv_freq[None, :]).astype(f32)
    ang_c = (col[:, None] * inv_freq[None, :]).astype(f32)
    tab = np.zeros((S, 2, 2, 16), dtype=f32)
    tab[:, 0, 0] = np.cos(ang_r)
    tab[:, 0, 1] = np.sin(ang_r)
    tab[:, 1, 0] = np.cos(ang_c)
    tab[:, 1, 1] = np.sin(ang_c)
    c["tab"] = np.ascontiguousarray(tab.reshape(NT, 128, 2, 2, 16).transpose(1, 0, 2, 3, 4))
    return c


def _pk(v):
    v = np.asarray(v, dtype=np.float32)
    lead = v.shape[:-1]
    a = v.reshape(lead + (8, 128))
    return np.ascontiguousarray(np.moveaxis(a, -1, 0))


_NC_CACHE = {}


def make_in_maps(x, norm_mix, norm_ffn, conv_in, conv_w, conv_out, attn_qkv, attn_q_norm,
                 attn_k_norm, attn_out, router, w_gate, w_up, w_down, final_norm):
    f32 = np.float32
    c = _constants()
    shared = dict(c)
    shared["conv_in"] = np.ascontiguousarray(conv_in[0], dtype=f32)
    shared["conv_out"] = np.ascontiguousarray(conv_out[0], dtype=f32)
    shared["attn_qkv"] = np.ascontiguousarray(attn_qkv[0], dtype=f32)
    shared["attn_out"] = np.ascontiguousarray(attn_out[0], dtype=f32)
    shared["w_gate"] = np.ascontiguousarray(w_gate, dtype=f32)
    shared["w_up"] = np.ascontiguousarray(w_up, dtype=f32)
    shared["w_down"] = np.ascontiguousarray(w_down, dtype=f32)
    shared["gm"] = _pk(norm_mix)
    shared["gf"] = _pk(norm_ffn)
    shared["gfin"] = np.ascontiguousarray(np.broadcast_to(np.asarray(final_norm, dtype=f32)[None, :], (128, D)))
    shared["cw"] = _pk(conv_w[0]).transpose(0, 2, 1).copy()
    r = np.asarray(router, dtype=f32).reshape(2, 8, 128, NE)
    shared["rt"] = np.ascontiguousarray(r.transpose(2, 0, 1, 3))
    shared["gq"] = np.ascontiguousarray(np.broadcast_to(np.asarray(attn_q_norm[0], dtype=f32)[None, :], (128, 64)))
    shared["gk"] = np.ascontiguousarray(np.broadcast_to(np.asarray(attn_k_norm[0], dtype=f32)[None, :], (128, 64)))
    maps = []
    for b in range(8):
        m = dict(shared)
        m["x"] = np.ascontiguousarray(x[b], dtype=f32)
        maps.append(m)
    return maps


def kernel(x, norm_mix, norm_ffn, conv_in, conv_w, conv_out, attn_qkv, attn_q_norm,
           attn_k_norm, attn_out, router, w_gate, w_up, w_down, final_norm):
    if "nc" not in _NC_CACHE:
        _NC_CACHE["nc"] = build_program()
    nc = _NC_CACHE["nc"]
    maps = make_in_maps(x, norm_mix, norm_ffn, conv_in, conv_w, conv_out, attn_qkv, attn_q_norm,
                        attn_k_norm, attn_out, router, w_gate, w_up, w_down, final_norm)
    res = run_bass_kernel_spmd(nc, maps, core_ids=list(range(8)))
    return np.stack([np.asarray(r["out"], dtype=np.float32) for r in res.results], axis=0)
```

```python
import os
import numpy as np
import ml_dtypes
from contextlib import ExitStack
import concourse.bass as bass
import concourse.mybir as mybir
from concourse.bass_utils import run_bass_kernel_spmd

F32 = mybir.dt.float32
BF16 = mybir.dt.bfloat16
I32 = mybir.dt.int32
AF = mybir.ActivationFunctionType
ALU = mybir.AluOpType
AX = mybir.AxisListType

S = 4096
D = 1024
NT = 32
NE = 16
CAP = 512
FF = 2048
EPS = 1e-6
ENGS = ("pe", "act", "dve", "pool", "sp")
STAGE = 99
DEBUG = False
CUT = int(os.environ.get('CUT', '99'))


class Buf:
    __slots__ = ("name", "w", "r", "sem", "dma_total")

    def __init__(self, name):
        self.name = name
        self.w = {}
        self.r = {}
        self.sem = None
        self.dma_total = 0


class Tile:
    def __init__(self, t, name):
        self.t = t
        self.b = Buf(name)

    def __getitem__(self, idx):
        return self.t[idx]


def _b(x):
    return x.b if isinstance(x, Tile) else x


class Prog:
    def __init__(self, nc, stack):
        self.nc = nc
        self.stack = stack
        self.q = {e: [] for e in ENGS}
        self.cnt = {e: 0 for e in ENGS}
        self.esem = {e: stack.enter_context(nc.semaphore("es_" + e)) for e in ENGS}
        self.waited = {e: {} for e in ENGS}
        self.nsem = len(ENGS)
        self.nops = 0
        self.uid = 0
        self.owners = {}

    def name(self, base):
        self.uid += 1
        return "%s_%d" % (base, self.uid)

    def sb(self, st, name, shape, dt):
        return Tile(st.enter_context(self.nc.sbuf_tensor(self.name(name), list(shape), dt)), name)

    def _bufsem(self, b):
        if b.sem is None:
            b.sem = self.stack.enter_context(self.nc.semaphore(self.name("ds_" + b.name)))
            self.nsem += 1
        return b.sem

    def _collect(self, eng, reads, writes, is_dma):
        deps = {}
        own = id(self.esem[eng])

        def add(d, skip_own):
            for k, (s, v) in d.items():
                if skip_own and k == own:
                    continue
                if k not in deps or deps[k][1] < v:
                    deps[k] = (s, v)
        for b in reads:
            add(_b(b).w, skip_own=(eng == "pe") and not is_dma)
        for b in writes:
            add(_b(b).w, skip_own=not is_dma)
            add(_b(b).r, skip_own=not is_dma)
        out = []
        wd = self.waited[eng]
        for k, (s, v) in deps.items():
            if wd.get(k, 0) < v:
                wd[k] = v
                out.append((s, v))
        return out

    def op(self, eng, fn, reads=(), writes=()):
        waits = self._collect(eng, reads, writes, False)
        self.cnt[eng] += 1
        sem = self.esem[eng]
        self.q[eng].append((waits, fn, sem, 1))
        ev = (sem, self.cnt[eng])
        k = id(sem)
        for b in reads:
            _b(b).r[k] = ev
        for b in writes:
            _b(b).w[k] = ev
        self.nops += 1

    def dma(self, eng, fn, reads=(), writes=(), nodep_writes=(), owner=None):
        waits = self._collect(eng, reads, writes, True)
        tgt = _b(owner if owner is not None else (list(writes) + list(nodep_writes))[0])
        sem = self._bufsem(tgt)
        tgt.dma_total += 16
        self.owners[id(tgt)] = tgt
        self.q[eng].append((waits, fn, sem, 16))
        ev = (sem, tgt.dma_total)
        k = id(sem)
        for b in reads:
            _b(b).r[k] = ev
        for b in list(writes) + list(nodep_writes):
            _b(b).w[k] = ev
        self.nops += 1

    def wait_all(self, eng, bufs):
        waits = self._collect(eng, (), bufs, True)
        self.q[eng].append((waits, None, None, 0))

    def emit(self):
        nc = self.nc
        names = {"pe": "tensor", "act": "scalar", "dve": "vector", "pool": "gpsimd", "sp": "sync"}
        if not any(self.q[e] for e in ENGS):
            return
        wd = self.waited["sp"]
        drain = []
        for tgt in self.owners.values():
            k = id(tgt.sem)
            if wd.get(k, 0) < tgt.dma_total:
                wd[k] = tgt.dma_total
                drain.append((tgt.sem, tgt.dma_total))
        self.q["sp"].append((drain, None, None, 0))
        with nc.Block() as block:
            for e in ENGS:
                lst = self.q[e]

                def body(E, lst=lst):
                    for waits, fn, sem, inc in lst:
                        for (s, v) in waits:
                            E.wait_ge(s, v)
                        if fn is not None:
                            fn(E).then_inc(sem, inc)
                getattr(block, names[e])(body)
        self.q = {e: [] for e in ENGS}


class Ring:
    def __init__(self, items):
        self.items = items
        self.i = 0

    def next(self):
        x = self.items[self.i % len(self.items)]
        self.i += 1
        return x


def build_program(stage=STAGE, debug=DEBUG, only=None, ngroups=8):
    nc = bass.Bass("TRN2", target_bir_lowering=False)

    def din(name, shape, dt=F32):
        return nc.dram_tensor(name, list(shape), dt, kind="ExternalInput").ap()

    x_d = din("x", [S, D])
    conv_in_d = din("conv_in", [D, 3 * D])
    conv_out_d = din("conv_out", [D, D])
    qkv_d = din("attn_qkv", [D, 1536])
    wo_d = din("attn_out", [D, D])
    wg_d = din("w_gate", [2, NE, D, FF])
    wu_d = din("w_up", [2, NE, D, FF])
    wd_d = din("w_down", [2, NE, FF, D])
    gm_d = din("gm", [128, 2, 8])
    gf_d = din("gf", [128, 2, 8])
    gfin_d = din("gfin", [128, D])
    cw_d = din("cw", [128, 8, 3])
    rt_d = din("rt", [128, 2, 8, NE])
    gq_d = din("gq", [128, 64])
    gk_d = din("gk", [128, 64])
    tab_d = din("tab", [128, NT, 2, 2, 16])
    id32_d = din("id32", [128, 128])
    idbf_d = din("idbf", [128, 128], BF16)
    iota1_d = din("iota1", [128, CAP])
    pj_d = din("pj", [128, NT, NE, 2], BF16)
    gmat_d = din("gmat", [128, 128])
    lmat_d = din("lmat", [128, 128])

    okind = "ExternalOutput"
    out_d = nc.dram_tensor("out", [S, D], F32, kind=okind).ap()
    ikind = "ExternalOutput" if debug else "Internal"
    hA_d = nc.dram_tensor("hA", [S, D], F32, kind=ikind).ap()
    hB_d = nc.dram_tensor("hB", [S, D], F32, kind=ikind).ap()
    xn2_d = nc.dram_tensor("xn2", [S, D], BF16, kind=ikind).ap()
    qs_d = nc.dram_tensor("qscr", [S, D], BF16, kind=ikind).ap()
    if debug:
        dbg_log = nc.dram_tensor("dbg_log", [128, NT, NE], F32, kind=okind).ap()
        dbg_idx = nc.dram_tensor("dbg_idx", [128, NE, 4], I32, kind=okind).ap()
        dbg_gate = nc.dram_tensor("dbg_gate", [128, NE, 4], F32, kind=okind).ap()

    with ExitStack() as top:
        P = Prog(nc, top)

        def mm(out, lhsT, rhs, start, stop, R, W):
            P.op("pe", lambda E: E.matmul(out, lhsT, rhs, start=start, stop=stop), R, W)

        def tr(out, in_, ident, R, W):
            P.op("pe", lambda E: E.transpose(out, in_, ident), R, W)

        def act(out, in_, func, R, W, scale=None, bias=None, accum=None):
            kw = {}
            if scale is not None:
                kw["scale"] = scale
            if bias is not None:
                kw["bias"] = bias
            if accum is not None:
                kw["accum_out"] = accum
            P.op("act", lambda E: E.activation(out, in_, func, **kw), R, W)

        def ts(eng, out, in0, s1, s2, op0, op1, R, W):
            if op1 is None:
                P.op(eng, lambda E: E.tensor_scalar(out, in0, s1, None, op0), R, W)
            else:
                P.op(eng, lambda E: E.tensor_scalar(out, in0, s1, s2, op0, op1), R, W)

        def tt(eng, out, in0, in1, op, R, W):
            P.op(eng, lambda E: E.tensor_tensor(out, in0, in1, op), R, W)

        def stt(out, in0, scalar, in1, op0, op1, R, W):
            P.op("dve", lambda E: E.scalar_tensor_tensor(out, in0, scalar, in1, op0, op1), R, W)

        def dma(q, out, in_, R, W, owner=None, **kw):
            P.dma(q, lambda E: E.dma_start(out=out, in_=in_, **kw), R, W, owner=owner)

        gm = P.sb(top, "gm", [128, 2, 8], F32)
        gf = P.sb(top, "gf", [128, 2, 8], F32)
        cw = P.sb(top, "cw", [128, 8, 3], F32)
        rt = P.sb(top, "rt", [128, 2, 8, NE], F32)
        id32 = P.sb(top, "id32", [128, 128], F32)
        idbf = P.sb(top, "idbf", [128, 128], BF16)
        logits = P.sb(top, "logits", [128, NT, NE], F32)
        for tl, src in ((gm, gm_d), (gf, gf_d), (cw, cw_d), (rt, rt_d), (id32, id32_d), (idbf, idbf_d)):
            dma("sp", tl.t[:], src, (), (tl,))

        banks = [Tile(top.enter_context(nc.psum_tensor("bank%d" % i, [128, 512], F32)), "bank%d" % i)
                 for i in range(8)]

        def bk_bf(i):
            return banks[i].t[:, :].bitcast(BF16)

        x_b = Buf("x")
        sc_bufs = [Buf("sc0"), Buf("sc1")]
        hA_b = [Buf("hA0"), Buf("hA1")]
        hB_b = [Buf("hB0"), Buf("hB1")]
        xn2_b = Buf("xn2")
        qs_b = Buf("qscr")
        out_b = Buf("out")

        def rstd_from_ss(st_tiles, ss, n, scale_extra=1.0, inv_dim=1.0 / D):
            ms, sd, rs = st_tiles
            ts("dve", ms.t[0:n, :], ss.t[0:n, :], inv_dim, EPS, ALU.mult, ALU.add, (ss,), (ms,))
            act(sd.t[0:n, :], ms.t[0:n, :], AF.Sqrt, (ms,), (sd,))
            P.op("dve", lambda E: E.reciprocal(rs.t[0:n, :], sd.t[0:n, :]), (sd,), (rs,))
            return rs

        def run_skewed(gens_, offs):
            m = len(gens_)
            nst = len(offs)
            for sl in range(m + offs[-1]):
                for k in reversed(range(nst)):
                    i = sl - offs[k]
                    if 0 <= i < m:
                        try:
                            next(gens_[i])
                        except StopIteration:
                            pass

        def post_mixer(pm, t, po_banks, res_src, res_bufs, dst, dst_bufs, layer, tb0, tb1, r_pre=None):
            rows = slice(t * 128, (t + 1) * 128)
            if r_pre is not None:
                r = r_pre
            else:
                r = pm["r"].next()
                dma("sp", r.t[:], res_src[rows, :], tuple(res_bufs), (r,))
            hn = pm["hn"].next()
            for hf in range(2):
                cs = slice(hf * 512, (hf + 1) * 512)
                tt("dve", hn.t[:, cs], po_banks[hf].t[:, :], r.t[:, cs], ALU.add, (po_banks[hf], r), (hn,))
            P.dma("sp", lambda E: E.dma_start(out=dst[rows, :], in_=hn.t[:]), (hn,), (), nodep_writes=tuple(dst_bufs), owner=hn)
            junk = pm["junk"]
            ss = pm["ss"].next()
            act(junk.t[:], hn.t[:], AF.Square, (hn,), (junk, ss), accum=ss.t[:, :])
            rs = rstd_from_ss(pm["st"].next(), ss, 128)
            xo = pm["xo"].next()
            act(xo.t[:], hn.t[:], AF.Copy, (hn, rs), (xo,), scale=rs.t[:, 0:1])
            P.dma("sp", lambda E: E.dma_start(out=xn2_d[rows, :], in_=xo.t[:]), (xo,), (), nodep_writes=(xn2_b,), owner=xo)
            hT = pm["hT"].next()
            for hf in range(2):
                tb = (tb0, tb1)[hf]
                for kk in range(4):
                    k = hf * 4 + kk
                    tr(tb.t[:, kk * 128:(kk + 1) * 128], hn.t[:, k * 128:(k + 1) * 128], id32.t[:], (hn, id32), (tb,))
                tt("dve", hT.t[:, hf * 4:(hf + 1) * 4, :],
                   tb.t[:, :].rearrange("p (k c) -> p k c", k=4),
                   gf.t[:, layer, hf * 4:(hf + 1) * 4].unsqueeze(2).to_broadcast([128, 4, 128]),
                   ALU.mult, (tb, gf), (hT,))
            for k in range(8):
                mm(tb0.t[:, 0:NE], hT.t[:, k, :], rt.t[:, layer, k, :], k == 0, k == 7, (hT, rt), (tb0,))
            ts("dve", logits.t[:, t, :], tb0.t[:, 0:NE], rs.t[:, 0:1], None, ALU.mult, None, (tb0, rs), (logits,))

        def alloc_post_mixer(st):
            pm = {}
            pm["r"] = Ring([P.sb(st, "pm_r", [128, D], F32) for _ in range(4)])
            pm["hn"] = Ring([P.sb(st, "pm_hn", [128, D], F32) for _ in range(3)])
            pm["junk"] = P.sb(st, "pm_junk", [128, D], BF16)
            pm["ss"] = Ring([P.sb(st, "pm_ss", [128, 1], F32) for _ in range(2)])
            pm["st"] = Ring([[P.sb(st, "pm_st", [128, 1], F32) for _ in range(3)] for _ in range(8)])
            pm["xo"] = Ring([P.sb(st, "pm_xo", [128, D], BF16) for _ in range(2)])
            pm["hT"] = Ring([P.sb(st, "pm_hT", [128, 8, 128], F32) for _ in range(3)])
            return pm

        def phase_conv():
            with ExitStack() as st:
                wp = [P.sb(st, "cwp", [128, 8, 1024], BF16) for _ in range(4)]
                for i in range(3):
                    dma("pool", wp[i].t[:], conv_in_d[:, i * 1024:(i + 1) * 1024].rearrange("(k p) f -> p k f", p=128), (), (wp[i],))
                dma("pool", wp[3].t[:], conv_out_d.rearrange("(k p) f -> p k f", p=128), (), (wp[3],))
                Wb, Wc, Wh, Wo = wp
                pm = alloc_post_mixer(st)
                xts = Ring([P.sb(st, "xt", [128, D], F32) for _ in range(4)])
                xns = Ring([P.sb(st, "xn", [128, D], BF16) for _ in range(3)])
                junk = P.sb(st, "cjunk", [128, D], BF16)
                sss = Ring([P.sb(st, "css", [128, 1], F32) for _ in range(2)])
                sts = Ring([[P.sb(st, "cst", [128, 1], F32) for _ in range(3)] for _ in range(2)])
                xnTs = [P.sb(st, "xnT", [128, 8, 1026], BF16)] * 2
                zTs = [P.sb(st, "zT", [128, 8, 1024], BF16)] * 2
                us = Ring([P.sb(st, "u", [128, 1026], F32) for _ in range(2)])
                ctmp = Ring([P.sb(st, "ctmp", [128, 512], F32) for _ in range(2)])
                ys = Ring([P.sb(st, "y", [128, 512], F32) for _ in range(2)])
                trb = Ring([0, 1])
                pcb = Ring([2, 3])
                phb = Ring([4, 5])
                pbb = Ring([6, 7])

                def norm_gen(xnT, r0, n, col0):
                    xt = xts.next()
                    dma("sp", xt.t[0:n, :], x_d[r0:r0 + n, :], (x_b,), (xt,))
                    yield
                    ss = sss.next()
                    act(junk.t[0:n, :], xt.t[0:n, :], AF.Square, (xt,), (junk, ss), accum=ss.t[0:n, :])
                    rs = rstd_from_ss(sts.next(), ss, n)
                    xn = xns.next()
                    ts("dve", xn.t[0:n, :], xt.t[0:n, :], rs.t[0:n, 0:1], None, ALU.mult, None, (xt, rs), (xn,))
                    yield
                    bi = trb.next()
                    pv = bk_bf(bi)
                    for k in range(8):
                        tr(pv[:, k * 128:k * 128 + n], xn.t[0:n, k * 128:(k + 1) * 128], idbf.t[0:n, 0:n], (xn, idbf), (banks[bi],))
                    yield
                    tt("dve", xnT.t[:, :, col0:col0 + n],
                       pv.rearrange("p (k c) -> p k c", k=8)[:, :, 0:n],
                       gm.t[:, 0, :].unsqueeze(2).to_broadcast([128, 8, n]),
                       ALU.mult, (banks[bi], gm), (xnT,))

                def norm_rows_all(specs):
                    run_skewed([norm_gen(*sp) for sp in specs], [0, 2, 3, 4])

                def norm_specs(qi):
                    lo = qi * 1024
                    hi = lo + 1024
                    xq = xnTs[qi % 2]
                    specs = []
                    if lo > 0:
                        specs.append((xq, lo - 1, 1, 0))
                    for tl in range(8):
                        specs.append((xq, lo + tl * 128, 128, 1 + tl * 128))
                    if hi < S:
                        specs.append((xq, hi, 1, 1025))
                    return specs

                def channel_j(qi, j):
                    lo = qi * 1024
                    hi = lo + 1024
                    xnT = xnTs[qi % 2]
                    zT = zTs[qi % 2]
                    c_lo = 0 if lo > 0 else 1
                    c_hi = 1026 if hi < S else 1025
                    groups = []
                    c = c_lo
                    while c < c_hi:
                        n = min(512, c_hi - c)
                        groups.append((c, n))
                        c += n
                    js = slice(j * 128, (j + 1) * 128)
                    u = us.next()
                    if c_lo == 1:
                        P.op("pool", lambda E, u=u: E.memset(u.t[:, 0:1], 0.0), (), (u,))
                    if c_hi == 1025:
                        P.op("pool", lambda E, u=u: E.memset(u.t[:, 1025:1026], 0.0), (), (u,))
                    for (c0, n) in groups:
                        pc = banks[pcb.next()]
                        ph = banks[phb.next()]
                        for k in range(8):
                            mm(pc.t[:, 0:n], Wc.t[:, k, js], xnT.t[:, k, c0:c0 + n], k == 0, k == 7, (Wc, xnT), (pc,))
                        for k in range(8):
                            mm(ph.t[:, 0:n], Wh.t[:, k, js], xnT.t[:, k, c0:c0 + n], k == 0, k == 7, (Wh, xnT), (ph,))
                        ct = ctmp.next()
                        act(ct.t[:, 0:n], pc.t[:, 0:n], AF.Copy, (pc,), (ct,))
                        tt("dve", u.t[:, c0:c0 + n], ct.t[:, 0:n], ph.t[:, 0:n], ALU.mult, (ct, ph), (u,))
                    for g in range(2):
                        pb = banks[pbb.next()]
                        for k in range(8):
                            mm(pb.t[:, :], Wb.t[:, k, js], xnT.t[:, k, 1 + g * 512:1 + (g + 1) * 512], k == 0, k == 7, (Wb, xnT), (pb,))
                        y = ys.next()
                        a = g * 512
                        ts("dve", y.t[:], u.t[:, a:a + 512], cw.t[:, j, 0:1], None, ALU.mult, None, (u, cw), (y,))
                        stt(y.t[:], u.t[:, a + 1:a + 513], cw.t[:, j, 1:2], y.t[:], ALU.mult, ALU.add, (u, cw, y), (y,))
                        stt(y.t[:], u.t[:, a + 2:a + 514], cw.t[:, j, 2:3], y.t[:], ALU.mult, ALU.add, (u, cw, y), (y,))
                        tt("dve", zT.t[:, j, a:a + 512], y.t[:], pb.t[:, :], ALU.mult, (y, pb), (zT,))

                lgb = Ring([6, 7])

                def post_gen(qi, tl):
                    zT = zTs[qi % 2]
                    t = qi * 8 + tl
                    rows = slice(t * 128, (t + 1) * 128)
                    r = pm["r"].next()
                    dma("sp", r.t[:], x_d[rows, :], (x_b,), (r,))
                    yield
                    pob = (banks[2 + (tl % 2) * 2], banks[3 + (tl % 2) * 2])
                    for hf in range(2):
                        for k in range(8):
                            mm(pob[hf].t[:, :], zT.t[:, k, tl * 128:(tl + 1) * 128], Wo.t[:, k, hf * 512:(hf + 1) * 512],
                               k == 0, k == 7, (zT, Wo), (pob[hf],))
                    yield
                    hn = pm["hn"].next()
                    for hf in range(2):
                        cs = slice(hf * 512, (hf + 1) * 512)
                        tt("dve", hn.t[:, cs], pob[hf].t[:, :], r.t[:, cs], ALU.add, (pob[hf], r), (hn,))
                    P.dma("sp", lambda E: E.dma_start(out=hA_d[rows, :], in_=hn.t[:]), (hn,), (), nodep_writes=tuple(hA_b), owner=hn)
                    pjunk = pm["junk"]
                    ss = pm["ss"].next()
                    act(pjunk.t[:], hn.t[:], AF.Square, (hn,), (pjunk, ss), accum=ss.t[:, :])
                    rs = rstd_from_ss(pm["st"].next(), ss, 128)
                    xo = pm["xo"].next()
                    act(xo.t[:], hn.t[:], AF.Copy, (hn, rs), (xo,), scale=rs.t[:, 0:1])
                    P.dma("sp", lambda E: E.dma_start(out=xn2_d[rows, :], in_=xo.t[:]), (xo,), (), nodep_writes=(xn2_b,), owner=xo)
                    yield
                    tbs = (banks[0], banks[1])
                    for hf in range(2):
                        for kk in range(4):
                            k = hf * 4 + kk
                            tr(tbs[hf].t[:, kk * 128:(kk + 1) * 128], hn.t[:, k * 128:(k + 1) * 128], id32.t[:], (hn, id32), (tbs[hf],))
                    yield
                    hT = pm["hT"].next()
                    for hf in range(2):
                        tt("dve", hT.t[:, hf * 4:(hf + 1) * 4, :], tbs[hf].t[:, :].rearrange("p (k c) -> p k c", k=4),
                           gf.t[:, 0, hf * 4:(hf + 1) * 4].unsqueeze(2).to_broadcast([128, 4, 128]), ALU.mult, (tbs[hf], gf), (hT,))
                    yield
                    lb = banks[lgb.next()]
                    for k in range(8):
                        mm(lb.t[:, 0:NE], hT.t[:, k, :], rt.t[:, 0, k, :], k == 0, k == 7, (hT, rt), (lb,))
                    yield
                    ts("dve", logits.t[:, t, :], lb.t[:, 0:NE], rs.t[:, 0:1], None, ALU.mult, None, (lb, rs), (logits,))

                def post_steps_q(qi):
                    return [lambda: run_skewed([post_gen(qi, tl) for tl in range(8)], [0, 2, 3, 4, 5, 6, 7])]

                for qi in range(4):
                    norm_rows_all(norm_specs(qi))
                    for j in range(8):
                        channel_j(qi, j)
                    for st_ in post_steps_q(qi):
                        st_()
                P.emit()

        def phase_moe(layer, h_d, h_bufs):
            with ExitStack() as st:
                NR = 8
                ring = Ring([P.sb(st, "wr", [128, 8, 1024], BF16) for _ in range(NR)])
                idx = P.sb(st, "idx", [128, NE, 4], I32)
                gates = P.sb(st, "gates", [128, NE, 4], F32)
                pmk = P.sb(st, "pmk", [128, NT, NE], F32)
                tv = P.sb(st, "tv", [128, NT, NE, 5], BF16)
                iota1 = P.sb(st, "iota1", [128, CAP], F32)
                ohs = Ring([P.sb(st, "oh", [128, CAP], BF16) for _ in range(4)])
                idxT = P.sb(st, "idxT", [8, CAP], F32)
                idxf = P.sb(st, "idxf", [128, 4, 5], F32)
                IB = 7

                def oh_dve(e, j):
                    oh = ohs.next()
                    ts("dve", oh.t[:], iota1.t[:], pmk.t[:, j, e:e + 1], None, ALU.is_equal, None, (iota1, pmk), (oh,))
                    return oh

                def oh_pe(e, j, oh):
                    mm(banks[IB].t[0:5, :], tv.t[:, j, e, :], oh.t[:], j == 0, j == NT - 1, (oh, tv), (banks[IB],))

                def oh_finish(e):
                    P.op("dve", lambda E: E.tensor_copy(idxT.t[0:5, :], banks[IB].t[0:5, :]), (banks[IB],), (idxT,))
                    for c in range(4):
                        tr(banks[IB].t[:, c * 8:c * 8 + 5], idxT.t[0:5, c * 128:(c + 1) * 128], id32.t[0:5, 0:5], (idxT, id32), (banks[IB],))
                    P.op("dve", lambda E: E.tensor_copy(idxf.t[:], banks[IB].t[:, 0:32].rearrange("p (c f) -> p c f", c=4)[:, :, 0:5]),
                         (banks[IB],), (idxf,))
                    stt(idx.t[:, e, :], idxf.t[:, :, 1], 128.0, idxf.t[:, :, 0], ALU.mult, ALU.add, (idxf,), (idx,))
                    P.op("dve", lambda E, e=e: E.tensor_reduce(gates.t[:, e, :], idxf.t[:, :, 2:5], AX.X, ALU.add), (idxf,), (gates,))

                def load_piece(src):
                    w = ring.next()
                    dma("pool", w.t[:], src, (), (w,))
                    return w

                def expert_pieces(e, which):
                    if which == "g":
                        return [load_piece(wg_d[layer, e][:, hf * 1024:(hf + 1) * 1024].rearrange("(k p) f -> p k f", p=128)) for hf in range(2)]
                    if which == "u":
                        return [load_piece(wu_d[layer, e][:, hf * 1024:(hf + 1) * 1024].rearrange("(k p) f -> p k f", p=128)) for hf in range(2)]
                    return [load_piece(wd_d[layer, e][hf * 1024:(hf + 1) * 1024, :].rearrange("(k p) f -> p k f", p=128)) for hf in range(2)]

                def half(wd_, e, hf):
                    return wd_[layer, e][:, hf * 1024:(hf + 1) * 1024].rearrange("(k p) f -> p k f", p=128)
                g0_ = load_piece(half(wg_d, 0, 0))
                u0_ = load_piece(half(wu_d, 0, 0))
                g1_ = load_piece(half(wg_d, 0, 1))
                u1_ = load_piece(half(wu_d, 0, 1))
                w_d0 = expert_pieces(0, "d")
                cur_w = {"g": [g0_, g1_], "u": [u0_, u1_], "d": w_d0}

                with ExitStack() as rs_:
                    ex = P.sb(rs_, "ex", [128, NT, NE], F32)
                    aff = P.sb(rs_, "aff", [128, NT, NE], F32)
                    sm = P.sb(rs_, "sm", [128, NT], F32)
                    rsm = P.sb(rs_, "rsm", [128, NT], F32)
                    affS = P.sb(rs_, "affS", [128, 512], F32)
                    maskS = P.sb(rs_, "maskS", [128, 512], F32)
                    cjunk = P.sb(rs_, "cjunk", [128, 512], BF16)
                    mid = P.sb(rs_, "mid", [128, 1], F32)
                    thr = P.sb(rs_, "thr", [128, 1], F32)
                    cnt = P.sb(rs_, "cnt", [128, 1], F32)
                    tmpc = P.sb(rs_, "tmpc", [128, 1], F32)
                    one128 = P.sb(rs_, "one128", [128, 1], F32)
                    gmat = P.sb(rs_, "gmat", [128, 128], F32)
                    lmat = P.sb(rs_, "lmat", [128, 128], F32)
                    dma("sp", gmat.t[:], gmat_d, (), (gmat,))
                    dma("sp", lmat.t[:], lmat_d, (), (lmat,))
                    r1 = P.sb(rs_, "r1", [128, NT, NE], F32)
                    hi32 = P.sb(rs_, "hi32", [128, NT, NE], F32)

                    dma("sp", iota1.t[:], iota1_d, (), (iota1,))
                    pjc = P.sb(rs_, "pjc", [128, NT, NE, 2], BF16)
                    dma("sp", pjc.t[:], pj_d, (), (pjc,))
                    P.op("dve", lambda E: E.tensor_copy(tv.t[:, :, :, 0:2], pjc.t[:]), (pjc,), (tv,))
                    P.op("pool", lambda E: E.memset(one128.t[:], 1.0), (), (one128,))
                    act(ex.t[:], logits.t[:], AF.Exp, (logits,), (ex,))
                    P.op("dve", lambda E: E.tensor_reduce(sm.t[:], ex.t[:], AX.X, ALU.add), (ex,), (sm,))
                    P.op("dve", lambda E: E.reciprocal(rsm.t[:], sm.t[:]), (sm,), (rsm,))
                    tt("dve", aff.t[:], ex.t[:], rsm.t[:, :].unsqueeze(2).to_broadcast([128, NT, NE]), ALU.mult, (ex, rsm), (aff,))
                    P.op("dve", lambda E: E.tensor_copy(tv.t[:, :, :, 2], aff.t[:]), (aff,), (tv,))
                    P.op("dve", lambda E: E.tensor_copy(hi32.t[:], tv.t[:, :, :, 2]), (tv,), (hi32,))
                    tt("dve", r1.t[:], aff.t[:], hi32.t[:], ALU.subtract, (aff, hi32), (r1,))
                    P.op("dve", lambda E: E.tensor_copy(tv.t[:, :, :, 3], r1.t[:]), (r1,), (tv,))
                    P.op("dve", lambda E: E.tensor_copy(hi32.t[:], tv.t[:, :, :, 3]), (tv,), (hi32,))
                    tt("dve", r1.t[:], r1.t[:], hi32.t[:], ALU.subtract, (r1, hi32), (r1,))
                    P.op("dve", lambda E: E.tensor_copy(tv.t[:, :, :, 4], r1.t[:]), (r1,), (tv,))
                    tb_ = banks[0]
                    for b4 in range(4):
                        tr(tb_.t[:, b4 * 128:(b4 + 1) * 128], aff.t[:, b4 * 8:(b4 + 1) * 8, :].rearrange("p j e -> p (j e)"), id32.t[:],
                           (aff, id32), (tb_,))
                    act(affS.t[:], tb_.t[:, :], AF.Copy, (tb_,), (affS,))
                    P.op("pool", lambda E: E.memset(mid.t[:], 0.5), (), (mid,))
                    NIT = 24
                    for kq in range(NIT):
                        w = 2.0 ** -(kq + 1)
                        P.op("dve", lambda E: E.tensor_scalar(cjunk.t[:], affS.t[:], mid.t[:, 0:1], None, ALU.is_ge, ALU.add, accum_out=cnt.t[:, 0:1]),
                             (affS, mid), (cjunk, cnt))
                        mm(banks[1].t[:, 0:1], gmat.t[:], cnt.t[:, 0:1], True, True, (gmat, cnt), (banks[1],))
                        ts("dve", tmpc.t[:], banks[1].t[:, 0:1], 511.5, w, ALU.is_ge, ALU.mult, (banks[1],), (tmpc,))
                        stt(mid.t[:], tmpc.t[:], -w / 2, mid.t[:], ALU.add, ALU.add, (tmpc, mid), (mid,))
                    ts("dve", thr.t[:], mid.t[:], -(2.0 ** -(NIT + 1)), None, ALU.add, None, (mid,), (thr,))
                    P.op("dve", lambda E: E.tensor_scalar(maskS.t[:], affS.t[:], thr.t[:, 0:1], None, ALU.is_ge, ALU.add, accum_out=cnt.t[:, 0:1]),
                         (affS, thr), (maskS, cnt))
                    P.op("dve", lambda E: E.tensor_tensor_scan(out=affS.t[:], data0=one128.t[:, 0:1].to_broadcast([128, 512]), data1=maskS.t[:],
                                                              initial=0.0, op0=ALU.mult, op1=ALU.add), (one128, maskS), (affS,))
                    mm(banks[1].t[:, 0:1], lmat.t[:], cnt.t[:, 0:1], True, True, (lmat, cnt), (banks[1],))
                    P.op("dve", lambda E: E.tensor_copy(tmpc.t[:], banks[1].t[:, 0:1]), (banks[1],), (tmpc,))
                    stt(affS.t[:], affS.t[:], tmpc.t[:, 0:1], maskS.t[:], ALU.add, ALU.mult, (affS, tmpc, maskS), (affS,))
                    pmb = banks[0]
                    for b4 in range(4):
                        tr(pmb.t[:, b4 * 128:(b4 + 1) * 128], affS.t[:, b4 * 128:(b4 + 1) * 128], id32.t[:], (affS, id32), (pmb,))
                    P.op("dve", lambda E: E.tensor_copy(pmk.t[:], pmb.t[:, :].rearrange("p (j e) -> p j e", j=NT)), (pmb,), (pmk,))
                    for e in range(2 if stage >= 3 else NE):
                        for j in range(NT):
                            oh_pe(e, j, oh_dve(e, j))
                        oh_finish(e)
                    if debug and stage < 3:
                        dma("sp", dbg_idx, idx.t[:], (idx,), (Buf("dbgi"),))
                        dma("sp", dbg_gate, gates.t[:], (gates,), (Buf("dbgg"),))
                        dma("sp", dbg_log, logits.t[:], (logits,), (Buf("dbgl"),))
                    P.emit()

                if stage < 3:
                    P.emit()
                    return
                with ExitStack() as es_:
                    xgs = Ring([P.sb(es_, "xg", [128, 4, D], BF16) for _ in range(2)])
                    xgTs = Ring([P.sb(es_, "xgT", [128, 8, CAP], BF16) for _ in range(2)])
                    hT = P.sb(es_, "hTe", [128, 16, CAP], BF16)
                    sgs = Ring([P.sb(es_, "sg", [128, CAP], F32) for _ in range(2)])
                    ygs = Ring([P.sb(es_, "yg", [128, D], F32) for _ in range(2)])
                    pgb = Ring([0, 1])
                    pub = Ring([2, 3])
                    pyb = Ring([4, 5])
                    ptb = Ring([6])

                    def gather(e):
                        xg = xgs.next()
                        for c in range(4):
                            P.dma("pool", lambda E, c=c, xg=xg: E.indirect_dma_start(
                                out=xg.t[:, c, :], out_offset=None, in_=xn2_d[:, :],
                                in_offset=bass.IndirectOffsetOnAxis(ap=idx.t[:, e, c:c + 1], axis=0)),
                                (idx, xn2_b), (xg,) if c == 0 else (), nodep_writes=() if c == 0 else (xg,))
                        return xg

                    def transposes(xg):
                        xgT = xgTs.next()
                        for k in range(8):
                            bi = ptb.next()
                            pv = bk_bf(bi)
                            for c in range(4):
                                tr(pv[:, c * 128:(c + 1) * 128], xg.t[:, c, k * 128:(k + 1) * 128], idbf.t[:], (xg, idbf), (banks[bi],))
                            if k % 2 == 0:
                                ts("dve", xgT.t[:, k, :], pv[:, 0:CAP], gf.t[:, layer, k:k + 1], None, ALU.mult, None, (banks[bi], gf), (xgT,))
                            else:
                                act(xgT.t[:, k, :], pv[:, 0:CAP], AF.Copy, (banks[bi], gf), (xgT,), scale=gf.t[:, layer, k:k + 1])
                        return xgT

                    xg_cur = gather(0)
                    xgT_cur = transposes(xg_cur)
                    xg_next = gather(1)
                    sc_prev = h_bufs
                    for e in range(NE):
                        nxt = {}
                        if e + 1 < NE:
                            nxt["g"] = [load_piece(wg_d[layer, e + 1][:, 0:1024].rearrange("(k p) f -> p k f", p=128)), None]
                            nxt["u"] = [load_piece(wu_d[layer, e + 1][:, 0:1024].rearrange("(k p) f -> p k f", p=128)), None]
                        for fc in range(16):
                            ohl = [oh_dve(e + 2, 2 * fc), oh_dve(e + 2, 2 * fc + 1)] if e + 2 < NE else []
                            wgp = cur_w["g"][fc // 8]
                            wup = cur_w["u"][fc // 8]
                            fs = slice((fc % 8) * 128, (fc % 8 + 1) * 128)
                            pg = banks[pgb.next()]
                            pu = banks[pub.next()]
                            for k in range(8):
                                mm(pg.t[:, :], wgp.t[:, k, fs], xgT_cur.t[:, k, :], k == 0, k == 7, (wgp, xgT_cur), (pg,))
                            for k in range(8):
                                mm(pu.t[:, :], wup.t[:, k, fs], xgT_cur.t[:, k, :], k == 0, k == 7, (wup, xgT_cur), (pu,))
                            sg = sgs.next()
                            act(sg.t[:], pg.t[:, :], AF.Silu, (pg,), (sg,))
                            tt("dve", hT.t[:, fc, :], sg.t[:], pu.t[:, :], ALU.mult, (sg, pu), (hT,))
                            for jj_, oh_ in enumerate(ohl):
                                oh_pe(e + 2, 2 * fc + jj_, oh_)
                            if e + 1 < NE and fc == 7:
                                nxt["g"][1] = load_piece(wg_d[layer, e + 1][:, 1024:2048].rearrange("(k p) f -> p k f", p=128))
                                nxt["u"][1] = load_piece(wu_d[layer, e + 1][:, 1024:2048].rearrange("(k p) f -> p k f", p=128))
                        if e + 1 < NE:
                            nxt["d"] = expert_pieces(e + 1, "d")
                            xgT_next = transposes(xg_next)
                            if e + 2 < NE:
                                oh_finish(e + 2)
                                xg_next = gather(e + 2)
                        sc_cur = [sc_bufs[e % 2]]
                        for c in range(4):
                            yg = ygs.next()
                            for hf in range(2):
                                py = banks[pyb.next()]
                                for fc in range(16):
                                    wdp = cur_w["d"][fc // 8]
                                    mm(py.t[:, :], hT.t[:, fc, c * 128:(c + 1) * 128], wdp.t[:, fc % 8, hf * 512:(hf + 1) * 512],
                                       fc == 0, fc == 15, (hT, wdp), (py,))
                                if hf == 0:
                                    act(yg.t[:, 0:512], py.t[:, :], AF.Copy, (py, gates), (yg,), scale=gates.t[:, e, c:c + 1])
                                else:
                                    ts("dve", yg.t[:, 512:1024], py.t[:, :], gates.t[:, e, c:c + 1], None, ALU.mult, None, (py, gates), (yg,))
                            P.dma("pool", lambda E, c=c, yg=yg, e=e: E.indirect_dma_start(
                                out=h_d[:, :], out_offset=bass.IndirectOffsetOnAxis(ap=idx.t[:, e, c:c + 1], axis=0),
                                in_=yg.t[:], in_offset=None, compute_op=ALU.add),
                                [idx, yg] + list(sc_prev), (), nodep_writes=tuple(sc_cur), owner=yg)
                        sc_prev = sc_cur
                        if e + 1 < NE:
                            cur_w = nxt
                            xgT_cur = xgT_next
                    h_bufs[:] = list(h_bufs) + sc_prev
                    P.emit()

        def phase_attn():
            with ExitStack() as st:
                kTA = P.sb(st, "kTA", [128, 4, S], BF16)
                kTB = P.sb(st, "kTB", [128, 4, S], BF16)
                vx = P.sb(st, "vx", [128, NT, 4, 65], BF16)
                with ExitStack() as s1:
                    wq = P.sb(s1, "wq", [128, 8, 1024], BF16)
                    wkv = P.sb(s1, "wkv", [128, 8, 512], BF16)
                    dma("pool", wq.t[:], qkv_d[:, 0:1024].rearrange("(k p) f -> p k f", p=128), (), (wq,))
                    dma("pool", wkv.t[:], qkv_d[:, 1024:1536].rearrange("(k p) f -> p k f", p=128), (), (wkv,))
                    for k in range(8):
                        ts("dve", wq.t[:, k, :], wq.t[:, k, :], gm.t[:, 1, k:k + 1], None, ALU.mult, None, (wq, gm), (wq,))
                        act(wkv.t[:, k, :], wkv.t[:, k, :], AF.Copy, (wkv, gm), (wkv,), scale=gm.t[:, 1, k:k + 1])
                    tab = P.sb(s1, "tab", [128, NT, 2, 2, 16], F32)
                    gq = P.sb(s1, "gq", [128, 64], F32)
                    gk = P.sb(s1, "gk", [128, 64], F32)
                    dma("sp", tab.t[:], tab_d, (), (tab,))
                    dma("sp", gq.t[:], gq_d, (), (gq,))
                    dma("sp", gk.t[:], gk_d, (), (gk,))
                    P.op("pool", lambda E: E.memset(vx.t[:, :, :, 64:65], 1.0), (), (vx,))
                    xts = Ring([P.sb(s1, "axt", [128, D], F32) for _ in range(4)])
                    xns = Ring([P.sb(s1, "axn", [128, D], BF16) for _ in range(2)])
                    junk = P.sb(s1, "ajunk", [128, D], BF16)
                    sss = Ring([P.sb(s1, "ass", [128, 1], F32) for _ in range(2)])
                    sts = Ring([[P.sb(s1, "ast", [128, 1], F32) for _ in range(3)] for _ in range(4)])
                    xnTs = Ring([P.sb(s1, "axnT", [128, 8, 128], BF16) for _ in range(2)])
                    sq2 = [P.sb(s1, "sq", [128, 1280], F32) for _ in range(2)]
                    ssh2 = [P.sb(s1, "ssh", [128, 20], F32) for _ in range(2)]
                    hms2 = [P.sb(s1, "hms", [128, 20], F32) for _ in range(2)]
                    hsd2 = [P.sb(s1, "hsd", [128, 20], F32) for _ in range(2)]
                    hrs2 = [P.sb(s1, "hrs", [128, 20], F32) for _ in range(2)]
                    tq2 = [P.sb(s1, "tq", [128, 4, 2, 16], F32) for _ in range(2)]
                    tk2 = [P.sb(s1, "tk", [128, 4, 2, 16], F32) for _ in range(2)]
                    ab2 = [[P.sb(s1, "ab%d" % i, [128, 20, 2, 16], F32) for i in range(4)] for _ in range(2)]
                    rop2 = [P.sb(s1, "rop", [128, 20, 2, 2, 16], F32) for _ in range(2)]
                    qrs = Ring([P.sb(s1, "qr", [128, D], BF16) for _ in range(2)])
                    kds = Ring([P.sb(s1, "kd", [128, 2, 4, 2, 64], BF16) for _ in range(2)])
                    for kd_ in kds.items:
                        P.op(os.environ.get("KDENG", "pool"), lambda E, kd_=kd_: E.memset(kd_.t[:], 0.0), (), (kd_,))
                    mhalf1 = P.sb(s1, "mhalf1", [128, 1], F32)
                    P.op("pool", lambda E: E.memset(mhalf1.t[:], -0.5), (), (mhalf1,))
                    trb = Ring([0, 1])
                    pqb = Ring([(2, 3, 4), (5, 6, 7)])
                    gqv = gq.t[:, :].rearrange("p (a h f) -> p a h f", a=2, h=2)
                    gkv = gk.t[:, :].rearrange("p (a h f) -> p a h f", a=2, h=2)
                    def tile_ops(j):
                        rows = slice(j * 128, (j + 1) * 128)
                        xt = xts.next()
                        dma("sp", xt.t[:], hA_d[rows, :], tuple(hA_b), (xt,))
                        yield
                        ss = sss.next()
                        act(junk.t[:], xt.t[:], AF.Square, (xt,), (junk, ss), accum=ss.t[:, :])
                        ms, r2, rs = sts.next()
                        ts("dve", ms.t[:], ss.t[:], 1.0 / D, EPS, ALU.mult, ALU.add, (ss,), (ms,))
                        tt("pool", rs.t[:], ms.t[:], mhalf1.t[:], ALU.pow, (ms, mhalf1), (rs,))
                        stt(r2.t[:], rs.t[:], 1.0 / 64, rs.t[:], ALU.mult, ALU.mult, (rs,), (r2,))
                        xn = xns.next()
                        act(xn.t[:], xt.t[:], AF.Copy, (xt,), (xn,))
                        bi = trb.next()
                        pv = bk_bf(bi)
                        for k in range(8):
                            tr(pv[:, k * 128:(k + 1) * 128], xn.t[:, k * 128:(k + 1) * 128], idbf.t[:], (xn, idbf), (banks[bi],))
                        xnT = xnTs.next()
                        act(xnT.t[:], pv.rearrange("p (k c) -> p k c", k=8), AF.Copy, (banks[bi],), (xnT,))
                        yield
                        b3 = pqb.next()
                        for gi, (w, c0) in enumerate(((wq, 0), (wq, 512), (wkv, 0))):
                            pb = banks[b3[gi]]
                            for k in range(8):
                                mm(pb.t[:, :], xnT.t[:, k, :], w.t[:, k, c0:c0 + 512], k == 0, k == 7, (xnT, w), (pb,))
                        yield
                        rs_tok, r2_tok = rs, r2
                        jp = j % 2
                        sq, ssh, hms, hsd, hrs = sq2[jp], ssh2[jp], hms2[jp], hsd2[jp], hrs2[jp]
                        tq, tk, ab, rop = tq2[jp], tk2[jp], ab2[jp], rop2[jp]
                        pq0, pq1, pkv = banks[b3[0]], banks[b3[1]], banks[b3[2]]
                        act(vx.t[:, j, :, 0:64], pkv.t[:, 256:512].rearrange("p (h d) -> p h d", h=4), AF.Copy, (pkv, rs_tok), (vx,),
                            scale=rs_tok.t[:, 0:1])
                        act(sq.t[:, 0:512], pq0.t[:, :], AF.Square, (pq0,), (sq,))
                        act(sq.t[:, 512:1024], pq1.t[:, :], AF.Square, (pq1,), (sq,))
                        act(sq.t[:, 1024:1280], pkv.t[:, 0:256], AF.Square, (pkv,), (sq,))
                        P.op("dve", lambda E: E.tensor_reduce(ssh.t[:], sq.t[:, :].rearrange("p (h d) -> p h d", d=64), AX.X, ALU.add), (sq,), (ssh,))
                        ts("dve", hms.t[:], ssh.t[:], r2_tok.t[:, 0:1], EPS, ALU.mult, ALU.add, (ssh, r2_tok), (hms,))
                        act(hsd.t[:], hms.t[:], AF.Sqrt, (hms,), (hsd,))
                        P.op("dve", lambda E: E.reciprocal(hsd.t[:], hsd.t[:]), (hsd,), (hsd,))
                        ts("dve", hrs.t[:, 0:16], hsd.t[:, 0:16], rs_tok.t[:, 0:1], 0.125, ALU.mult, ALU.mult, (hsd, rs_tok), (hrs,))
                        ts("dve", hrs.t[:, 16:20], hsd.t[:, 16:20], rs_tok.t[:, 0:1], None, ALU.mult, None, (hsd, rs_tok), (hrs,))
                        cosv = tab.t[:, j, :, 0, :]
                        sinv = tab.t[:, j, :, 1, :]
                        for (tbl, gv) in ((tq, gqv), (tk, gkv)):
                            tt("pool", tbl.t[:, 0], cosv, gv[:, :, 0, :], ALU.mult, (tab, gq, gk), (tbl,))
                            tt("pool", tbl.t[:, 1], sinv, gv[:, :, 1, :], ALU.mult, (tab, gq, gk), (tbl,))
                            tt("pool", tbl.t[:, 2], cosv, gv[:, :, 1, :], ALU.mult, (tab, gq, gk), (tbl,))
                            tt("pool", tbl.t[:, 3], sinv, gv[:, :, 0, :], ALU.mult, (tab, gq, gk), (tbl,))
                        for (pb, h0, nh, tbl) in ((pq0, 0, 8, tq), (pq1, 8, 8, tq), (pkv, 16, 4, tk)):
                            v = pb.t[:, 0:nh * 64].rearrange("p (h a x f) -> p h a x f", h=nh, a=2, x=2)
                            x1 = v[:, :, :, 0, :]
                            x2 = v[:, :, :, 1, :]
                            hs = slice(h0, h0 + nh)

                            def bc(i):
                                return tbl.t[:, i].unsqueeze(1).to_broadcast([128, nh, 2, 16])
                            tt("dve", ab[0].t[:, hs], x1, bc(0), ALU.mult, (pb, tbl), (ab[0],))
                            tt("dve", ab[1].t[:, hs], x2, bc(1), ALU.mult, (pb, tbl), (ab[1],))
                            tt("dve", ab[2].t[:, hs], x2, bc(2), ALU.mult, (pb, tbl), (ab[2],))
                            tt("dve", ab[3].t[:, hs], x1, bc(3), ALU.mult, (pb, tbl), (ab[3],))
                        yield
                        tt("pool", rop.t[:, :, :, 0, :], ab[0].t[:], ab[1].t[:], ALU.subtract, (ab[0], ab[1]), (rop,))
                        tt("pool", rop.t[:, :, :, 1, :], ab[2].t[:], ab[3].t[:], ALU.add, (ab[2], ab[3]), (rop,))
                        qr = qrs.next()
                        kd = kds.next()
                        ropv = rop.t[:, :, :, :, :].rearrange("p h a x f -> p h (a x f)")
                        tt("dve", qr.t[:, :].rearrange("p (h d) -> p h d", d=64), ropv[:, 0:16, :],
                           hrs.t[:, 0:16].unsqueeze(2).to_broadcast([128, 16, 64]), ALU.mult, (rop, hrs), (qr,))
                        for ab_ in range(2):
                            tt("dve", kd.t[:, ab_, :, ab_, :], ropv[:, 16:20, :],
                               hrs.t[:, 16:20].unsqueeze(2).to_broadcast([128, 4, 64]), ALU.mult, (rop, hrs), (kd,))
                        P.dma("sp", lambda E, rows=rows, qr=qr: E.dma_start(out=qs_d[rows, :], in_=qr.t[:]), (qr,), (), nodep_writes=(qs_b,), owner=qr)
                        yield
                        bi = trb.next()
                        pv = bk_bf(bi)
                        for ab_ in range(2):
                            for h in range(4):
                                tr(pv[:, (ab_ * 4 + h) * 128:(ab_ * 4 + h + 1) * 128], kd.t[:, ab_, h, :, :].rearrange("p a d -> p (a d)"),
                                   idbf.t[:], (kd, idbf), (banks[bi],))
                        act(kTA.t[:, :, rows], pv[:, 0:512].rearrange("p (h c) -> p h c", h=4), AF.Copy, (banks[bi],), (kTA,))
                        act(kTB.t[:, :, rows], pv[:, 512:1024].rearrange("p (h c) -> p h c", h=4), AF.Copy, (banks[bi],), (kTB,))

                    gens = {}

                    def adv(t):
                        if 0 <= t < NT:
                            try:
                                next(gens[t])
                            except StopIteration:
                                pass
                    for t0_ in range(2):
                        gens[t0_] = tile_ops(t0_)
                        adv(t0_)
                    for sl in range(NT + 5):
                        if sl + 2 < NT:
                            gens[sl + 2] = tile_ops(sl + 2)
                            adv(sl + 2)
                        adv(sl - 4)
                        adv(sl - 2)
                        adv(sl - 3)
                        adv(sl - 1)
                        adv(sl)
                    P.emit()
                if stage < 5:
                    return
                with ExitStack() as s2:
                    wo = P.sb(s2, "wo", [128, 8, 1024], BF16)
                    dma("pool", wo.t[:], wo_d.rearrange("(k p) f -> p k f", p=128), (), (wo,))
                    pm = alloc_post_mixer(s2)
                    qgs = Ring([P.sb(s2, "qg", [128, 4, D], BF16) for _ in range(2)])
                    qTs = Ring([P.sb(s2, "qT", [128, 8, 512], BF16) for _ in range(2)])
                    pTs = Ring([P.sb(s2, "pT", [128, 512], BF16) for _ in range(4)])
                    aos = Ring([P.sb(s2, "ao", [128, 4, D], BF16) for _ in range(2)])
                    aoTs = Ring([P.sb(s2, "aoT", [128, 8, 128], BF16) for _ in range(2)])
                    rinvs = Ring([P.sb(s2, "rinv", [128, 1], F32) for _ in range(4)])
                    psb = Ring([0, 1, 6])

                    def load_q(g):
                        qg = qgs.next()
                        dma("sp", qg.t[:], qs_d[g * 512:(g + 1) * 512, :].rearrange("(c p) d -> p c d", p=128), (qs_b,), (qg,))
                        return qg

                    def q_transposes(qg):
                        qT = qTs.next()
                        for c in range(4):
                            bi = 7
                            pv = bk_bf(bi)
                            for pr in range(8):
                                tr(pv[:, pr * 128:(pr + 1) * 128], qg.t[:, c, pr * 128:(pr + 1) * 128], idbf.t[:], (qg, idbf), (banks[bi],))
                            P.op("dve", lambda E, c=c, pv=pv, qT=qT: E.tensor_copy(qT.t[:, :, c * 128:(c + 1) * 128], pv.rearrange("p (r c) -> p r c", r=8)),
                                 (banks[bi],), (qT,))
                        return qT

                    B7 = banks[7]
                    mhalf = P.sb(s2, "mhalf", [128, 1], F32)
                    P.op("pool", lambda E: E.memset(mhalf.t[:], -0.5), (), (mhalf,))

                    def q_steps(qg, qT):
                        steps = []
                        for c in range(4):
                            def step(c=c):
                                pv = bk_bf(7)
                                for pr in range(8):
                                    tr(pv[:, pr * 128:(pr + 1) * 128], qg.t[:, c, pr * 128:(pr + 1) * 128], idbf.t[:], (qg, idbf), (B7,))
                                P.op("dve", lambda E: E.tensor_copy(qT.t[:, :, c * 128:(c + 1) * 128], pv.rearrange("p (r c) -> p r c", r=8)),
                                     (B7,), (qT,))
                            steps.append(step)
                        return steps

                    def post_steps(g, ao):
                        steps = []
                        for qs in range(4):
                            t = g * 4 + qs
                            rows = slice(t * 128, (t + 1) * 128)
                            c = {}

                            def sA(qs=qs, rows=rows, c=c):
                                c["r"] = pm["r"].next()
                                dma("sp", c["r"].t[:], hA_d[rows, :], tuple(hA_b), (c["r"],))
                                pv = bk_bf(7)
                                for k in range(8):
                                    tr(pv[:, k * 128:(k + 1) * 128], ao.t[:, qs, k * 128:(k + 1) * 128], idbf.t[:], (ao, idbf), (B7,))
                                c["aoT"] = aoTs.next()
                                aoT = c["aoT"]
                                P.op("dve", lambda E: E.tensor_copy(aoT.t[:], pv.rearrange("p (k c) -> p k c", k=8)), (B7,), (aoT,))
                                c["hn"] = pm["hn"].next()

                            def sO(hf, rows=rows, c=c):
                                aoT, hn, r = c["aoT"], c["hn"], c["r"]
                                cs = slice(hf * 512, (hf + 1) * 512)
                                for k in range(8):
                                    mm(B7.t[:, :], aoT.t[:, k, :], wo.t[:, k, cs], k == 0, k == 7, (aoT, wo), (B7,))
                                tt("dve", hn.t[:, cs], B7.t[:, :], r.t[:, cs], ALU.add, (B7, r), (hn,))
                                if hf == 1:
                                    P.dma("sp", lambda E: E.dma_start(out=hB_d[rows, :], in_=hn.t[:]), (hn,), (), nodep_writes=tuple(hB_b), owner=hn)
                                    junk = pm["junk"]
                                    ss = pm["ss"].next()
                                    P.op("dve", lambda E: E.scalar_tensor_tensor(junk.t[:], hn.t[:], 1.0, hn.t[:], ALU.mult, ALU.mult, accum_out=ss.t[:, 0:1]),
                                         (hn,), (junk, ss))
                                    ms, _, rs = pm["st"].next()
                                    ts("dve", ms.t[:], ss.t[:], 1.0 / D, EPS, ALU.mult, ALU.add, (ss,), (ms,))
                                    tt("pool", rs.t[:], ms.t[:], mhalf.t[:], ALU.pow, (ms, mhalf), (rs,))
                                    c["rs"] = rs
                                    xo = pm["xo"].next()
                                    ts("dve", xo.t[:], hn.t[:], rs.t[:, 0:1], None, ALU.mult, None, (hn, rs), (xo,))
                                    P.dma("sp", lambda E: E.dma_start(out=xn2_d[rows, :], in_=xo.t[:]), (xo,), (), nodep_writes=(xn2_b,), owner=xo)
                                    c["hT"] = pm["hT"].next()

                            def sH(hf, c=c):
                                hn, hT = c["hn"], c["hT"]
                                for kk in range(4):
                                    k = hf * 4 + kk
                                    tr(B7.t[:, kk * 128:(kk + 1) * 128], hn.t[:, k * 128:(k + 1) * 128], id32.t[:], (hn, id32), (B7,))
                                tt("dve", hT.t[:, hf * 4:(hf + 1) * 4, :], B7.t[:, :].rearrange("p (k c) -> p k c", k=4),
                                   gf.t[:, 1, hf * 4:(hf + 1) * 4].unsqueeze(2).to_broadcast([128, 4, 128]), ALU.mult, (B7, gf), (hT,))

                            def sL(t=t, c=c):
                                hT, rs = c["hT"], c["rs"]
                                for k in range(8):
                                    mm(B7.t[:, 0:NE], hT.t[:, k, :], rt.t[:, 1, k, :], k == 0, k == 7, (hT, rt), (B7,))
                                ts("dve", logits.t[:, t, :], B7.t[:, 0:NE], rs.t[:, 0:1], None, ALU.mult, None, (B7, rs), (logits,))

                            steps += [sA, lambda sO=sO: sO(0), lambda sO=sO: sO(1), lambda sH=sH: sH(0), lambda sH=sH: sH(1), sL]
                        return steps

                    items = [(h, kt) for h in range(16) for kt in range(NT)]
                    qg0 = load_q(0)
                    qT_cur = qTs.next()
                    for st_ in q_steps(qg0, qT_cur):
                        st_()
                    pending = []
                    for g in range(ngroups):
                        qT = qT_cur
                        if g + 1 < ngroups:
                            qg_n = load_q(g + 1)
                            qT_next = qTs.next()
                            pending = pending + q_steps(qg_n, qT_next)
                        ao = aos.next()

                        def s_step(i):
                            h, kt = items[i]
                            pr, hf, kvh = h // 2, h % 2, h // 4
                            kTx = kTA if hf == 0 else kTB
                            psk = banks[psb.next()]
                            mm(psk.t[:, :], kTx.t[:, kvh, kt * 128:(kt + 1) * 128], qT.t[:, pr, :], True, True, (kTx, qT), (psk,))
                            pT = pTs.next()
                            act(pT.t[:], psk.t[:, :], AF.Exp, (psk,), (pT,))
                            return pT

                        LA = 2
                        pends = [s_step(i) for i in range(LA)]
                        for i in range(len(items)):
                            h, kt = items[i]
                            kvh = h // 4
                            if i + LA < len(items):
                                pends.append(s_step(i + LA))
                            pend = pends.pop(0)
                            for qs in range(4):
                                mm(banks[2 + qs].t[:, 0:65], pend.t[:, qs * 128:(qs + 1) * 128], vx.t[:, kt, kvh, :], kt == 0, kt == NT - 1,
                                   (pend, vx), (banks[2 + qs],))
                            if kt == NT - 1:
                                for qs in range(4):
                                    ri = rinvs.next()
                                    P.op("dve", lambda E, qs=qs, ri=ri: E.reciprocal(ri.t[:], banks[2 + qs].t[:, 64:65]), (banks[2 + qs],), (ri,))
                                    ts("dve", ao.t[:, qs, h * 64:(h + 1) * 64], banks[2 + qs].t[:, 0:64], ri.t[:, 0:1], None, ALU.mult, None,
                                       (banks[2 + qs], ri), (ao,))
                            if pending and i % 16 == 8:
                                pending.pop(0)()
                        while pending:
                            pending.pop(0)()
                        pending = post_steps(g, ao)
                        if g + 1 < ngroups:
                            qT_cur = qT_next
                    while pending:
                        pending.pop(0)()
                    P.emit()

        def phase_final(src_d, src_bufs):
            with ExitStack() as st:
                gfin = P.sb(st, "gfin", [128, D], F32)
                dma("sp", gfin.t[:], gfin_d, (), (gfin,))
                xts = Ring([P.sb(st, "fxt", [128, D], F32) for _ in range(5)])
                ys = Ring([P.sb(st, "fy", [128, D], F32) for _ in range(3)])
                junk = P.sb(st, "fjunk", [128, D], BF16)
                sss = Ring([P.sb(st, "fss", [128, 1], F32) for _ in range(2)])
                sts = Ring([[P.sb(st, "fst", [128, 1], F32) for _ in range(3)] for _ in range(3)])
                def fin_gen(t):
                    rows = slice(t * 128, (t + 1) * 128)
                    xt = xts.next()
                    dma("sp", xt.t[:], src_d[rows, :], tuple(src_bufs), (xt,))
                    yield
                    ss = sss.next()
                    act(junk.t[:], xt.t[:], AF.Square, (xt,), (junk, ss), accum=ss.t[:, :])
                    rs = rstd_from_ss(sts.next(), ss, 128)
                    yield
                    y = ys.next()
                    stt(y.t[:], xt.t[:], rs.t[:, 0:1], gfin.t[:], ALU.mult, ALU.mult, (xt, rs, gfin), (y,))
                    P.dma("sp", lambda E: E.dma_start(out=out_d[rows, :], in_=y.t[:]), (y,), (), nodep_writes=(out_b,), owner=y)
                run_skewed([fin_gen(t) for t in range(NT)], [0, 2, 3])
                P.wait_all("sp", [out_b])
                P.emit()

        if only is None or only == "conv":
            phase_conv()
        last_d, last_b = hA_d, hA_b
        if stage >= 2 and only is None:
            phase_moe(0, hA_d, hA_b)
        if stage >= 4:
            phase_attn()
        if stage >= 6 and only is None:
            phase_moe(1, hB_d, hB_b)
            last_d, last_b = hB_d, hB_b
        phase_final(last_d, last_b)
    return nc


def _constants():
    f32 = np.float32
    c = {}
    c["id32"] = np.eye(128, dtype=f32)
    c["idbf"] = np.eye(128, dtype=f32).astype(ml_dtypes.bfloat16)
    c["iota1"] = np.ascontiguousarray(np.broadcast_to(np.arange(1, CAP + 1, dtype=f32)[None, :], (128, CAP)))
    pj = np.zeros((128, NT, NE, 2), dtype=f32)
    pj[:, :, :, 0] = np.arange(128, dtype=f32)[:, None, None]
    pj[:, :, :, 1] = np.arange(NT, dtype=f32)[None, :, None]
    c["pj"] = pj.astype(ml_dtypes.bfloat16)
    pp = np.arange(128)
    same = (pp[:, None] % NE) == (pp[None, :] % NE)
    c["gmat"] = same.astype(f32)
    c["lmat"] = (same & ((pp[:, None] // NE) < (pp[None, :] // NE))).astype(f32)
    t = np.arange(S)
    row = (t // 64).astype(f32)
    col = (t % 64).astype(f32)
    inv_freq = (f32(10000.0) ** (-np.arange(0, 32, 2, dtype=f32) / f32(32))).astype(f32)
    ang_r = (row[:, None] * inv_freq[None, :]).astype(f32)
    ang_c = (col[:, None] * inv_freq[None, :]).astype(f32)
    tab = np.zeros((S, 2, 2, 16), dtype=f32)
    tab[:, 0, 0] = np.cos(ang_r)
    tab[:, 0, 1] = np.sin(ang_r)
    tab[:, 1, 0] = np.cos(ang_c)
    tab[:, 1, 1] = np.sin(ang_c)
    c["tab"] = np.ascontiguousarray(tab.reshape(NT, 128, 2, 2, 16).transpose(1, 0, 2, 3, 4))
    return c


def _pk(v):
    v = np.asarray(v, dtype=np.float32)
    lead = v.shape[:-1]
    a = v.reshape(lead + (8, 128))
    return np.ascontiguousarray(np.moveaxis(a, -1, 0))


_NC_CACHE = {}


def make_in_maps(x, norm_mix, norm_ffn, conv_in, conv_w, conv_out, attn_qkv, attn_q_norm,
                 attn_k_norm, attn_out, router, w_gate, w_up, w_down, final_norm):
    f32 = np.float32
    c = _constants()
    shared = dict(c)
    shared["conv_in"] = np.ascontiguousarray(conv_in[0], dtype=f32)
    shared["conv_out"] = np.ascontiguousarray(conv_out[0], dtype=f32)
    shared["attn_qkv"] = np.ascontiguousarray(attn_qkv[0], dtype=f32)
    shared["attn_out"] = np.ascontiguousarray(attn_out[0], dtype=f32)
    shared["w_gate"] = np.ascontiguousarray(w_gate, dtype=f32)
    shared["w_up"] = np.ascontiguousarray(w_up, dtype=f32)
    shared["w_down"] = np.ascontiguousarray(w_down, dtype=f32)
    shared["gm"] = _pk(norm_mix)
    shared["gf"] = _pk(norm_ffn)
    shared["gfin"] = np.ascontiguousarray(np.broadcast_to(np.asarray(final_norm, dtype=f32)[None, :], (128, D)))
    shared["cw"] = _pk(conv_w[0]).transpose(0, 2, 1).copy()
    r = np.asarray(router, dtype=f32).reshape(2, 8, 128, NE)
    shared["rt"] = np.ascontiguousarray(r.transpose(2, 0, 1, 3))
    shared["gq"] = np.ascontiguousarray(np.broadcast_to(np.asarray(attn_q_norm[0], dtype=f32)[None, :], (128, 64)))
    shared["gk"] = np.ascontiguousarray(np.broadcast_to(np.asarray(attn_k_norm[0], dtype=f32)[None, :], (128, 64)))
    maps = []
    for b in range(8):
        m = dict(shared)
        m["x"] = np.ascontiguousarray(x[b], dtype=f32)
        maps.append(m)
    return maps


def kernel(x, norm_mix, norm_ffn, conv_in, conv_w, conv_out, attn_qkv, attn_q_norm,
           attn_k_norm, attn_out, router, w_gate, w_up, w_down, final_norm):
    if "nc" not in _NC_CACHE:
        _NC_CACHE["nc"] = build_program()
    nc = _NC_CACHE["nc"]
    maps = make_in_maps(x, norm_mix, norm_ffn, conv_in, conv_w, conv_out, attn_qkv, attn_q_norm,
                        attn_k_norm, attn_out, router, w_gate, w_up, w_down, final_norm)
    res = run_bass_kernel_spmd(nc, maps, core_ids=list(range(8)))
    return np.stack([np.asarray(r["out"], dtype=np.float32) for r in res.results], axis=0)
```
